# Optimizing a Trainium2 kernel written in Bass

```python
import math
import jax, jax.numpy as jnp
from jax import lax
import numpy as np

D_MODEL = 1024
BATCH = 8
SEQ = 4096
DEPTH = 4

N_MEM = 256
NSA_HEADS = 8
NSA_KV_GROUPS = 2
NSA_HEAD_DIM = 64
NSA_CMP_LEN = 32
NSA_CMP_STRIDE = 16
NSA_CMP_HIDDEN = 256
NSA_SEL_LEN = 64
NSA_SEL_TOPK = 16
NSA_WINDOW = 512
NSA_Q_BLOCK = 64
NSA_Q_W = NSA_HEADS * NSA_HEAD_DIM
NSA_KV_W = NSA_KV_GROUPS * NSA_HEAD_DIM
RWKV_HEADS = 4
RWKV_HEAD_DIM = 64
RWKV_W = RWKV_HEADS * RWKV_HEAD_DIM
RWKV_DECAY_RANK = 64
RWKV_ICLR_RANK = 64
RWKV_GATE_RANK = 128
RWKV_LN_EPS = 64e-5
RWKV_SPLIT_SIZES = (RWKV_W, RWKV_W, RWKV_W, RWKV_DECAY_RANK, RWKV_ICLR_RANK, RWKV_GATE_RANK)
RWKV_SHIFT_W = 3 * RWKV_W + RWKV_DECAY_RANK + RWKV_ICLR_RANK + RWKV_GATE_RANK
S5_GROUPS = 16
S5_GROUP_WIDTH = 16
S5_STATE = 64
S5_W = S5_GROUPS * S5_GROUP_WIDTH
XA_HEADS = 4
XA_HEAD_DIM = 64
XA_W = XA_HEADS * XA_HEAD_DIM
D_FF = -(-8 * D_MODEL // (3 * 256)) * 256
IN_SPLIT_SIZES = (NSA_Q_W, 6 * NSA_KV_W, 3 * NSA_HEADS, RWKV_SHIFT_W, S5_W, 3 * D_MODEL)
IN_WIDTH = sum(IN_SPLIT_SIZES)
ROPE_THETA = 10000.0
NORM_EPS = 1e-6
NEG_INF = -1e30
SEL_FORCE_SCORE = 1e9

kernel_name = 'hybrid_nsa_rwkv7_s5_gated_trunk'


def _split_last(z, sizes):
    offsets = []
    acc = 0
    for size in sizes[:-1]:
        acc += size
        offsets.append(acc)
    return jnp.split(z, offsets, axis=-1)


def rmsnorm(x, g):
    xf = x.astype(jnp.float32)
    y = xf * lax.rsqrt(jnp.mean(xf * xf, axis=-1, keepdims=True) + NORM_EPS)
    return (y * g.astype(jnp.float32)).astype(x.dtype)


def rope_tables(seq, dim):
    inv = 1.0 / (ROPE_THETA ** (jnp.arange(0, dim, 2, dtype=jnp.float32) / dim))
    ang = jnp.arange(seq, dtype=jnp.float32)[:, None] * inv[None, :]
    return jnp.cos(ang), jnp.sin(ang)


def apply_rope(x, cos, sin):
    x1, x2 = jnp.split(x, 2, axis=-1)
    c = cos[None, :, None, :].astype(x.dtype)
    s = sin[None, :, None, :].astype(x.dtype)
    return jnp.concatenate([x1 * c - x2 * s, x1 * s + x2 * c], axis=-1)


def _compress_blocks(t, pos, w1, w2):
    B, S, G, hd = t.shape
    r = NSA_CMP_LEN // NSA_CMP_STRIDE
    chunks = t.reshape(B, S // NSA_CMP_STRIDE, NSA_CMP_STRIDE, G, hd)
    n_cmp = chunks.shape[1] - r + 1
    blocks = jnp.concatenate([chunks[:, i:i + n_cmp] for i in range(r)], axis=2)
    blocks = blocks + pos[:, None, :]
    flat = blocks.transpose(0, 1, 3, 2, 4).reshape(B, n_cmp, G, NSA_CMP_LEN * hd)
    return jax.nn.gelu(flat @ w1) @ w2


def nsa_attention(q, k_cmp, v_cmp, k_sel, v_sel, k_win, v_win, gates,
                  pos_k, pos_v, ck_w1, ck_w2, cv_w1, cv_w2, cos, sin):
    B, S, H, hd = q.shape
    G = NSA_KV_GROUPS
    R = H // G
    L = NSA_SEL_LEN
    QB = NSA_Q_BLOCK
    W = NSA_WINDOW
    n_sel = S // L
    top_k = min(NSA_SEL_TOPK, n_sel)
    scale = hd ** -0.5

    kc = _compress_blocks(k_cmp, pos_k, ck_w1, ck_w2)
    vc = _compress_blocks(v_cmp, pos_v, cv_w1, cv_w2)
    n_cmp = kc.shape[1]
    cmp_start = np.arange(n_cmp) * NSA_CMP_STRIDE
    sel_start = np.arange(n_sel) * L
    overlap = jnp.asarray(((cmp_start[:, None] < sel_start[None, :] + L) &
                           (cmp_start[:, None] + NSA_CMP_LEN > sel_start[None, :])).astype(np.float32))
    cmp_end = jnp.arange(n_cmp) * NSA_CMP_STRIDE + (NSA_CMP_LEN - 1)

    q_grp = q.reshape(B, S, G, R, hd)
    q_rot = apply_rope(q, cos, sin).reshape(B, S, G, R, hd)
    ks_blocks = apply_rope(k_sel, cos, sin).reshape(B, n_sel, L, G, hd).transpose(0, 3, 1, 2, 4)
    vs_blocks = v_sel.reshape(B, n_sel, L, G, hd).transpose(0, 3, 1, 2, 4)
    pad = ((0, 0), (W, 0), (0, 0), (0, 0))
    kw_pad = jnp.pad(apply_rope(k_win, cos, sin), pad)
    vw_pad = jnp.pad(v_win, pad)
    gates = gates.reshape(B, S, G, R, 3)
    b_ix = jnp.arange(B)[:, None, None]
    g_ix = jnp.arange(G)[None, :, None]
    blk = jnp.arange(n_sel)
    tok = jnp.arange(L)

    def query_block(c):
        t0 = c * QB
        t = t0 + jnp.arange(QB)
        qc = lax.dynamic_slice_in_dim(q_grp, t0, QB, axis=1)
        qr = lax.dynamic_slice_in_dim(q_rot, t0, QB, axis=1)
        gc = lax.dynamic_slice_in_dim(gates, t0, QB, axis=1)
        s = jnp.einsum('bqgrd,bngd->bgrqn', qc, kc).astype(jnp.float32) * scale
        ok = cmp_end[None, :] <= t[:, None]
        p_cmp = jax.nn.softmax(jnp.where(ok, s, NEG_INF), axis=-1) * (t >= NSA_CMP_LEN - 1)[:, None]
        o_cmp = jnp.einsum('bgrqn,bngd->bqgrd', p_cmp.astype(vc.dtype), vc)
        imp = jnp.einsum('bgrqn,nj->bgqj', p_cmp, overlap)
        cur = (t // L)[:, None]
        forced = (blk == 0) | (blk == cur) | (blk == cur - 1)
        imp = jnp.where(blk <= cur, jnp.where(forced, SEL_FORCE_SCORE, imp), NEG_INF)
        _, idx = lax.top_k(imp, top_k)
        idx_flat = idx.reshape(B, G, QB * top_k)
        ks = ks_blocks[b_ix, g_ix, idx_flat].reshape(B, G, QB, top_k, L, hd)
        vs = vs_blocks[b_ix, g_ix, idx_flat].reshape(B, G, QB, top_k, L, hd)
        key_pos = idx[..., None] * L + tok
        ok = key_pos <= t[:, None, None]
        s = jnp.einsum('bqgrd,bgqkld->bgrqkl', qr, ks).astype(jnp.float32) * scale
        s = jnp.where(ok[:, :, None], s, NEG_INF).reshape(B, G, R, QB, top_k * L)
        p = jax.nn.softmax(s, axis=-1).reshape(B, G, R, QB, top_k, L).astype(vs.dtype)
        o_sel = jnp.einsum('bgrqkl,bgqkld->bqgrd', p, vs)
        kw = lax.dynamic_slice_in_dim(kw_pad, t0, QB + W, axis=1)
        vw = lax.dynamic_slice_in_dim(vw_pad, t0, QB + W, axis=1)
        pos = t0 - W + jnp.arange(QB + W)
        ok = (pos[None, :] <= t[:, None]) & (pos[None, :] > t[:, None] - W) & (pos[None, :] >= 0)
        s = jnp.einsum('bqgrd,bkgd->bgrqk', qr, kw).astype(jnp.float32) * scale
        p = jax.nn.softmax(jnp.where(ok, s, NEG_INF), axis=-1).astype(vw.dtype)
        o_win = jnp.einsum('bgrqk,bkgd->bqgrd', p, vw)
        o = gc[..., 0:1] * o_cmp + gc[..., 1:2] * o_sel + gc[..., 2:3] * o_win
        return o.reshape(B, QB, H * hd)

    out = lax.map(query_block, jnp.arange(S // QB))
    return out.transpose(1, 0, 2, 3).reshape(B, S, H * hd)


def rwkv7_time_mix(z, mu, w0, w2, a0, a2, g2, k_k, k_a, r_k, ln_w, ln_b):
    B, S, _ = z.shape
    H, N = RWKV_HEADS, RWKV_HEAD_DIM
    z_prev = jnp.pad(z, ((0, 0), (1, 0), (0, 0)))[:, :-1]
    z = z + (z_prev - z) * mu
    r, k, v, w_lr, a_lr, g_lr = _split_last(z, RWKV_SPLIT_SIZES)
    w = -jax.nn.softplus(-(w0 + jnp.tanh(w_lr) @ w2)) - 0.5
    decay = jnp.exp(-jnp.exp(w.astype(jnp.float32)))
    a = jax.nn.sigmoid(a0 + a_lr @ a2)
    g = jax.nn.sigmoid(g_lr) @ g2

    def heads(t):
        return t.astype(jnp.float32).reshape(B, S, H, N)

    kk = heads(k * k_k)
    kk = kk / jnp.maximum(jnp.linalg.norm(kk, axis=-1, keepdims=True), 1e-12)
    k = k * (1.0 + (a - 1.0) * k_a)
    r, k, v, a, decay = heads(r), heads(k), heads(v), heads(a), heads(decay)

    def step(state, inp):
        r_t, k_t, v_t, kk_t, a_t, w_t = inp
        sa = jnp.einsum('bhvk,bhk->bhv', state, -kk_t)
        state = (state * w_t[:, :, None, :]
                 + sa[..., None] * (kk_t * a_t)[:, :, None, :]
                 + v_t[..., None] * k_t[:, :, None, :])
        return state, jnp.einsum('bhvk,bhk->bhv', state, r_t)

    xs = tuple(jnp.swapaxes(t, 0, 1) for t in (r, k, v, kk, a, decay))
    _, y = lax.scan(step, jnp.zeros((B, H, N, N), jnp.float32), xs)
    y = jnp.swapaxes(y, 0, 1)
    mean = jnp.mean(y, axis=-1, keepdims=True)
    var = jnp.mean(jnp.square(y - mean), axis=-1, keepdims=True)
    yn = ((y - mean) * lax.rsqrt(var + RWKV_LN_EPS)).reshape(B, S, RWKV_W) * ln_w + ln_b
    bonus = (jnp.sum(r * k * r_k.astype(jnp.float32), axis=-1, keepdims=True) * v).reshape(B, S, RWKV_W)
    return ((yn + bonus) * g).astype(z.dtype)


def _complex_affine_combine(e1, e2):
    a1r, a1i, b1r, b1i = e1
    a2r, a2i, b2r, b2i = e2
    ar = a1r * a2r - a1i * a2i
    ai = a1r * a2i + a1i * a2r
    br = a2r * b1r - a2i * b1i + b2r
    bi = a2r * b1i + a2i * b1r + b2i
    return ar, ai, br, bi


def s5_layer(u, lam_re, lam_im, log_dt, b_re, b_im, c_re, c_im, d, w_glu):
    B, S, _ = u.shape
    uf = u.astype(jnp.float32).reshape(B, S, S5_GROUPS, S5_GROUP_WIDTH)
    dt = jnp.exp(log_dt.astype(jnp.float32))[:, None]
    lr = lam_re.astype(jnp.float32)
    li = lam_im.astype(jnp.float32)
    mag = jnp.exp(lr * dt)
    abar_re = mag * jnp.cos(li * dt)
    abar_im = mag * jnp.sin(li * dt)
    den = lr * lr + li * li
    coef_re = ((abar_re - 1.0) * lr + abar_im * li) / den
    coef_im = (abar_im * lr - (abar_re - 1.0) * li) / den
    bu_re = jnp.einsum('bsgh,gph->bsgp', uf, b_re.astype(jnp.float32))
    bu_im = jnp.einsum('bsgh,gph->bsgp', uf, b_im.astype(jnp.float32))
    in_re = coef_re * bu_re - coef_im * bu_im
    in_im = coef_re * bu_im + coef_im * bu_re
    a_re = jnp.broadcast_to(abar_re, (1, S) + abar_re.shape)
    a_im = jnp.broadcast_to(abar_im, (1, S) + abar_im.shape)
    _, _, x_re, x_im = lax.associative_scan(_complex_affine_combine, (a_re, a_im, in_re, in_im), axis=1)
    y = (jnp.einsum('bsgp,ghp->bsgh', x_re, c_re.astype(jnp.float32))
         - jnp.einsum('bsgp,ghp->bsgh', x_im, c_im.astype(jnp.float32))
         + d.astype(jnp.float32) * uf)
    y = jax.nn.gelu(y.reshape(B, S, S5_W))
    y = y * jax.nn.sigmoid(y @ w_glu.astype(jnp.float32))
    return y.astype(u.dtype)


def hybrid_mixer(h, w_in, nsa_cmp_pos_k, nsa_cmp_pos_v, nsa_ck_w1, nsa_ck_w2, nsa_cv_w1, nsa_cv_w2,
                 rwkv_mu, rwkv_w0, rwkv_w2, rwkv_a0, rwkv_a2, rwkv_g2, rwkv_k_k, rwkv_k_a, rwkv_r_k,
                 rwkv_ln_w, rwkv_ln_b, s5_lam_re, s5_lam_im, s5_log_dt, s5_b_re, s5_b_im, s5_c_re,
                 s5_c_im, s5_d, s5_w_glu, w_up_nsa, w_up_rwkv, w_up_s5, w_out, cos, sin):
    B, S, _ = h.shape
    z = h @ w_in
    z_q, z_kv, z_ng, z_rwkv, z_s5, z_mg = _split_last(z, IN_SPLIT_SIZES)
    q = z_q.reshape(B, S, NSA_HEADS, NSA_HEAD_DIM)
    kvs = [t.reshape(B, S, NSA_KV_GROUPS, NSA_HEAD_DIM) for t in jnp.split(z_kv, 6, axis=-1)]
    nsa_gates = jax.nn.sigmoid(z_ng).reshape(B, S, NSA_HEADS, 3)
    y_nsa = nsa_attention(q, kvs[0], kvs[1], kvs[2], kvs[3], kvs[4], kvs[5], nsa_gates,
                          nsa_cmp_pos_k, nsa_cmp_pos_v, nsa_ck_w1, nsa_ck_w2, nsa_cv_w1, nsa_cv_w2, cos, sin)
    y_rwkv = rwkv7_time_mix(z_rwkv, rwkv_mu, rwkv_w0, rwkv_w2, rwkv_a0, rwkv_a2, rwkv_g2,
                            rwkv_k_k, rwkv_k_a, rwkv_r_k, rwkv_ln_w, rwkv_ln_b)
    y_s5 = s5_layer(z_s5, s5_lam_re, s5_lam_im, s5_log_dt, s5_b_re, s5_b_im, s5_c_re, s5_c_im, s5_d, s5_w_glu)
    g_nsa, g_rwkv, g_s5 = jnp.split(jax.nn.sigmoid(z_mg), 3, axis=-1)
    merged = g_nsa * (y_nsa @ w_up_nsa) + g_rwkv * (y_rwkv @ w_up_rwkv) + g_s5 * (y_s5 @ w_up_s5)
    return merged @ w_out


def cross_attention(h, m, w_q, w_k, w_v, w_o):
    B, S, _ = h.shape
    M = m.shape[1]
    q = (h @ w_q).reshape(B, S, XA_HEADS, XA_HEAD_DIM)
    k = (m @ w_k).reshape(B, M, XA_HEADS, XA_HEAD_DIM)
    v = (m @ w_v).reshape(B, M, XA_HEADS, XA_HEAD_DIM)
    s = jnp.einsum('bshd,bmhd->bhsm', q, k).astype(jnp.float32) * (XA_HEAD_DIM ** -0.5)
    p = jax.nn.softmax(s, axis=-1).astype(v.dtype)
    return jnp.einsum('bhsm,bmhd->bshd', p, v).reshape(B, S, XA_W) @ w_o


def swiglu_ffn(h, w_gate, w_up, w_down):
    return (jax.nn.silu(h @ w_gate) * (h @ w_up)) @ w_down


def setup_inputs(seed: int = 0) -> dict:
    key = jax.random.key(seed)
    split = jax.random.split(key, 64)
    keys = iter([split[i] for i in range(64)])

    def normal(shape, scale):
        return jax.random.normal(next(keys), shape, jnp.float32) * scale

    def uniform(shape, lo, hi):
        return jax.random.uniform(next(keys), shape, jnp.float32, lo, hi)

    L = DEPTH
    hd = NSA_HEAD_DIM
    ramp = (jnp.arange(RWKV_W, dtype=jnp.float32) / (RWKV_W - 1)) ** 0.7
    lam_im_base = math.pi * jnp.arange(S5_STATE, dtype=jnp.float32)
    return {
        'x': normal((BATCH, SEQ, D_MODEL), 1.0),
        'mem': normal((BATCH, N_MEM, D_MODEL), 1.0),
        'norm_gains': 1.0 + normal((L, 6, D_MODEL), 0.05),
        'mem_norm': 1.0 + normal((L, D_MODEL), 0.05),
        'w_in': normal((L, D_MODEL, IN_WIDTH), D_MODEL ** -0.5),
        'nsa_cmp_pos_k': normal((L, NSA_CMP_LEN, hd), 0.02),
        'nsa_cmp_pos_v': normal((L, NSA_CMP_LEN, hd), 0.02),
        'nsa_ck_w1': normal((L, NSA_CMP_LEN * hd, NSA_CMP_HIDDEN), (NSA_CMP_LEN * hd) ** -0.5),
        'nsa_ck_w2': normal((L, NSA_CMP_HIDDEN, hd), NSA_CMP_HIDDEN ** -0.5),
        'nsa_cv_w1': normal((L, NSA_CMP_LEN * hd, NSA_CMP_HIDDEN), (NSA_CMP_LEN * hd) ** -0.5),
        'nsa_cv_w2': normal((L, NSA_CMP_HIDDEN, hd), NSA_CMP_HIDDEN ** -0.5),
        'rwkv_mu': uniform((L, RWKV_SHIFT_W), 0.0, 1.0),
        'rwkv_w0': -6.0 + 5.0 * ramp + normal((L, RWKV_W), 0.1),
        'rwkv_w2': normal((L, RWKV_DECAY_RANK, RWKV_W), 0.1 * RWKV_DECAY_RANK ** -0.5),
        'rwkv_a0': normal((L, RWKV_W), 0.1),
        'rwkv_a2': normal((L, RWKV_ICLR_RANK, RWKV_W), RWKV_ICLR_RANK ** -0.5),
        'rwkv_g2': normal((L, RWKV_GATE_RANK, RWKV_W), RWKV_GATE_RANK ** -0.5),
        'rwkv_k_k': 0.85 + normal((L, RWKV_W), 0.02),
        'rwkv_k_a': 1.0 + normal((L, RWKV_W), 0.02),
        'rwkv_r_k': normal((L, RWKV_HEADS, RWKV_HEAD_DIM), 0.1),
        'rwkv_ln_w': 1.0 + normal((L, RWKV_W), 0.05),
        'rwkv_ln_b': normal((L, RWKV_W), 0.02),
        's5_lam_re': -0.5 + normal((L, S5_GROUPS, S5_STATE), 0.01),
        's5_lam_im': lam_im_base + normal((L, S5_GROUPS, S5_STATE), 0.01),
        's5_log_dt': uniform((L, S5_GROUPS), math.log(1e-3), math.log(1e-1)),
        's5_b_re': normal((L, S5_GROUPS, S5_STATE, S5_GROUP_WIDTH), (2 * S5_GROUP_WIDTH) ** -0.5),
        's5_b_im': normal((L, S5_GROUPS, S5_STATE, S5_GROUP_WIDTH), (2 * S5_GROUP_WIDTH) ** -0.5),
        's5_c_re': normal((L, S5_GROUPS, S5_GROUP_WIDTH, S5_STATE), S5_STATE ** -0.5),
        's5_c_im': normal((L, S5_GROUPS, S5_GROUP_WIDTH, S5_STATE), S5_STATE ** -0.5),
        's5_d': normal((L, S5_GROUPS, S5_GROUP_WIDTH), 1.0),
        's5_w_glu': normal((L, S5_W, S5_W), S5_W ** -0.5),
        'w_up_nsa': normal((L, NSA_Q_W, D_MODEL), NSA_Q_W ** -0.5),
        'w_up_rwkv': normal((L, RWKV_W, D_MODEL), RWKV_W ** -0.5),
        'w_up_s5': normal((L, S5_W, D_MODEL), S5_W ** -0.5),
        'w_out': normal((L, D_MODEL, D_MODEL), D_MODEL ** -0.5),
        'xa_w_q': normal((L, D_MODEL, XA_W), D_MODEL ** -0.5),
        'xa_w_k': normal((L, D_MODEL, XA_W), D_MODEL ** -0.5),
        'xa_w_v': normal((L, D_MODEL, XA_W), D_MODEL ** -0.5),
        'xa_w_o': normal((L, XA_W, D_MODEL), XA_W ** -0.5),
        'ffn_w_gate': normal((L, D_MODEL, D_FF), D_MODEL ** -0.5),
        'ffn_w_up': normal((L, D_MODEL, D_FF), D_MODEL ** -0.5),
        'ffn_w_down': normal((L, D_FF, D_MODEL), D_FF ** -0.5),
    }


def reference(x, mem, norm_gains, mem_norm, w_in, nsa_cmp_pos_k, nsa_cmp_pos_v, nsa_ck_w1, nsa_ck_w2,
              nsa_cv_w1, nsa_cv_w2, rwkv_mu, rwkv_w0, rwkv_w2, rwkv_a0, rwkv_a2, rwkv_g2, rwkv_k_k,
              rwkv_k_a, rwkv_r_k, rwkv_ln_w, rwkv_ln_b, s5_lam_re, s5_lam_im, s5_log_dt, s5_b_re,
              s5_b_im, s5_c_re, s5_c_im, s5_d, s5_w_glu, w_up_nsa, w_up_rwkv, w_up_s5, w_out,
              xa_w_q, xa_w_k, xa_w_v, xa_w_o, ffn_w_gate, ffn_w_up, ffn_w_down):
    cos, sin = rope_tables(x.shape[1], NSA_HEAD_DIM)
    for l in range(DEPTH):
        h = rmsnorm(x, norm_gains[l, 0])
        y = hybrid_mixer(h, w_in[l], nsa_cmp_pos_k[l], nsa_cmp_pos_v[l], nsa_ck_w1[l], nsa_ck_w2[l],
                         nsa_cv_w1[l], nsa_cv_w2[l], rwkv_mu[l], rwkv_w0[l], rwkv_w2[l], rwkv_a0[l],
                         rwkv_a2[l], rwkv_g2[l], rwkv_k_k[l], rwkv_k_a[l], rwkv_r_k[l], rwkv_ln_w[l],
                         rwkv_ln_b[l], s5_lam_re[l], s5_lam_im[l], s5_log_dt[l], s5_b_re[l], s5_b_im[l],
                         s5_c_re[l], s5_c_im[l], s5_d[l], s5_w_glu[l], w_up_nsa[l], w_up_rwkv[l],
                         w_up_s5[l], w_out[l], cos, sin)
        x = x + rmsnorm(y, norm_gains[l, 1])
        h = rmsnorm(x, norm_gains[l, 2])
        m = rmsnorm(mem, mem_norm[l])
        x = x + rmsnorm(cross_attention(h, m, xa_w_q[l], xa_w_k[l], xa_w_v[l], xa_w_o[l]), norm_gains[l, 3])
        h = rmsnorm(x, norm_gains[l, 4])
        x = x + rmsnorm(swiglu_ffn(h, ffn_w_gate[l], ffn_w_up[l], ffn_w_down[l]), norm_gains[l, 5])
    return x
```

```python
import math
import numpy as np
from contextlib import ExitStack
import concourse.bass as bass
import concourse.mybir as mybir
from concourse.bass_utils import run_bass_kernel_spmd

F32 = mybir.dt.float32
BF16 = mybir.dt.bfloat16
ALU = mybir.AluOpType
AF = mybir.ActivationFunctionType
AX = mybir.AxisListType

S_LEN = 4096
D = 1024
NT = 8
IN_W = 5656
BIG = 240000.0


class _Op:
    __slots__ = ("eng", "fn", "deps", "dma", "sig", "val", "ep")


class Sched:
    ENGS = ("pe", "act", "dve", "pool", "sp")
    DMA_K = 8

    def __init__(self, nc):
        self.nc = nc
        self.ops = []
        self.last_w = {}
        self.readers = {}
        self.dma_cnt = {}
        self.dma_hist = {}
        self.nfence = 0
        self.epoch = 0

    def add(self, eng, fn, reads=(), writes=(), dma=None):
        op = _Op()
        op.eng, op.fn, op.dma, op.sig, op.val = eng, fn, dma, False, 0
        op.ep = self.epoch
        deps = []
        if dma is not None:
            hist = self.dma_hist.setdefault(dma, [])
            idx = len(hist)
            if idx >= self.DMA_K:
                deps.append(hist[idx - self.DMA_K])
            hist.append(op)
            dma = op.dma = (dma, idx % self.DMA_K)
        for k in reads:
            w = self.last_w.get(k)
            if w is not None:
                deps.append(w)
        for k in writes:
            w = self.last_w.get(k)
            if w is not None:
                deps.append(w)
            deps.extend(self.readers.get(k, ()))
        out = []
        seen = set()
        for d in deps:
            if id(d) in seen:
                continue
            seen.add(id(d))
            if d.dma is None and dma is None and d.eng == eng == "pe":
                continue
            out.append(d)
        op.deps = out
        for k in reads:
            self.readers.setdefault(k, []).append(op)
        for k in writes:
            self.last_w[k] = op
            self.readers[k] = []
        if dma is not None:
            self.dma_cnt[dma] = self.dma_cnt.get(dma, 0) + 1
            op.val = self.dma_cnt[dma] * 16
            op.sig = True
        self.ops.append(op)
        return op

    def fence(self):
        n = self.nfence
        self.nfence += 1
        keys = list(set(self.last_w.keys()) | set(self.readers.keys()))
        for e in self.ENGS:
            self.add(e, lambda eng: eng.nop(), reads=keys, writes=[("fenceA", n, e)])
        self.last_w = {k: v for k, v in self.last_w.items() if isinstance(k, tuple) and k and k[0] == "fenceA" and k[1] == n}
        self.readers = {}
        fk = [("fenceA", n, e) for e in self.ENGS]
        for e in self.ENGS:
            self.add(e, lambda eng: eng.nop(), reads=fk, writes=[("fenceB", n, e)])
        self.last_w = {}
        self.readers = {}

    def new_epoch(self):
        self.epoch += 1

    def emit(self, final_dma_groups=()):
        nc = self.nc
        for op in self.ops:
            for d in op.deps:
                d.sig = True
        cnt = {}
        for op in self.ops:
            if op.dma is None and op.sig:
                k = (op.eng, op.ep)
                cnt[k] = cnt.get(k, 0) + 1
                op.val = cnt[k]
        per = {e: [o for o in self.ops if o.eng == e] for e in self.ENGS}
        final_dma_groups = [g for g in self.dma_cnt if g[0] in final_dma_groups]
        with ExitStack() as es:
            esem = {k: es.enter_context(nc.semaphore("s_%s_%d" % k)) for k in cnt}
            dsem = {g: es.enter_context(nc.semaphore("d_%s_%d" % g)) for g in self.dma_cnt}
            block = es.enter_context(nc.Block())

            def run(eng_name, e):
                wm = {}
                for op in per[eng_name]:
                    for d in op.deps:
                        if d.dma is not None:
                            s, key = dsem[d.dma], ("d", d.dma)
                        else:
                            s, key = esem[(d.eng, d.ep)], ("e", d.eng, d.ep)
                        if wm.get(key, 0) >= d.val:
                            continue
                        wm[key] = d.val
                        e.wait_ge(s, d.val)
                    ins = op.fn(e)
                    if op.sig:
                        if op.dma is not None:
                            ins.then_inc(dsem[op.dma], 16)
                        else:
                            ins.then_inc(esem[(op.eng, op.ep)], 1)
                if eng_name == "sp":
                    for g in final_dma_groups:
                        e.wait_ge(dsem[g], self.dma_cnt[g] * 16)

            @block.tensor
            def _(e):
                run("pe", e)

            @block.scalar
            def _(e):
                run("act", e)

            @block.vector
            def _(e):
                run("dve", e)

            @block.gpsimd
            def _(e):
                run("pool", e)

            @block.sync
            def _(e):
                run("sp", e)


C_Q, C_KCMP, C_VCMP, C_KSEL, C_VSEL, C_KWIN, C_VWIN = 0, 512, 640, 768, 896, 1024, 1152
C_RWKV, C_S5, C_MG, C_NG = 1280, 2304, 2560, 5632


class Builder:
    def __init__(self, L, dbg=(), stop_after=None, phases="ABCDEFG"):
        self.L = L
        self.phases = set(phases)
        self.dbg = set(dbg)
        self.stop_after = stop_after
        self.nc = bass.Bass("TRN2", target_bir_lowering=False)
        self.S = Sched(self.nc)
        self.ins = {}
        self.evq = 0

    def din(self, name, shape, dt=F32):
        t = self.nc.dram_tensor(name, list(shape), dt, kind="ExternalInput").ap()
        self.ins[name] = t
        return t

    def dscr(self, name, shape, dt):
        kind = "ExternalOutput" if name in self.dbg else "Internal"
        return self.nc.dram_tensor(name, list(shape), dt, kind=kind).ap()

    def dump(self, name, ap, shape, keys):
        if "dbgC" not in self.dbg:
            return
        d = self.nc.dram_tensor("dbg_" + name, list(shape), F32, kind="ExternalOutput").ap()
        self.dma(d, ap, keys, [("dbg", name)], "st")

    def ev_eng(self):
        self.evq += 1
        return "dve" if self.evq % 2 else "act"

    def dma(self, out, in_, reads, writes, grp, q="sp"):
        self.S.add(q, lambda e: e.dma_start(out=out, in_=in_), reads=reads, writes=writes, dma=grp)

    def mm(self, out, lhsT, rhs, start, stop, reads, writes):
        self.S.add("pe", lambda e: e.matmul(out, lhsT, rhs, start=start, stop=stop), reads=reads, writes=writes)

    def copy(self, out, in_, reads, writes, eng=None):
        eng = eng or self.ev_eng()
        if eng == "act":
            self.S.add("act", lambda e: e.copy(out=out, in_=in_), reads=reads, writes=writes)
        else:
            self.S.add(eng, lambda e: e.tensor_copy(out=out, in_=in_), reads=reads, writes=writes)

    def ts(self, out, in0, s1, s2, op0, op1=None, reads=(), writes=(), eng="dve"):
        if op1 is None:
            self.S.add(eng, lambda e: e.tensor_scalar(out=out, in0=in0, scalar1=s1, scalar2=None, op0=op0), reads=reads, writes=writes)
        else:
            self.S.add(eng, lambda e: e.tensor_scalar(out=out, in0=in0, scalar1=s1, scalar2=s2, op0=op0, op1=op1), reads=reads, writes=writes)

    def tt(self, out, in0, in1, op, reads, writes, eng="dve"):
        self.S.add(eng, lambda e: e.tensor_tensor(out=out, in0=in0, in1=in1, op=op), reads=reads, writes=writes)

    def stt(self, out, in0, scalar, in1, op0, op1, reads, writes, eng="dve"):
        self.S.add(eng, lambda e: e.scalar_tensor_tensor(out=out, in0=in0, scalar=scalar, in1=in1, op0=op0, op1=op1), reads=reads, writes=writes)

    def act(self, out, in_, func, reads, writes, scale=1.0, bias=0.0, accum_out=None):
        if accum_out is None:
            self.S.add("act", lambda e: e.activation(out=out, in_=in_, func=func, bias=bias, scale=scale), reads=reads, writes=writes)
        else:
            self.S.add("act", lambda e: e.activation(out=out, in_=in_, func=func, bias=bias, scale=scale, accum_out=accum_out), reads=reads, writes=writes)

    def rmsnorm_to_hT(self, es, xsrc, gcol, hT, tag, ntok=S_LEN):
        nc, S = self.nc, self.S
        ntile = ntok // 512 if ntok >= 512 else 1
        tw = min(512, ntok)
        ns = tw // 128
        xt = [es.enter_context(nc.sbuf_tensor(f"{tag}_xt{i}", [128, ns, 1024], F32)) for i in range(2)]
        xn = [es.enter_context(nc.sbuf_tensor(f"{tag}_xn{i}", [128, ns, 1024], BF16)) for i in range(2)]
        junk = es.enter_context(nc.sbuf_tensor(f"{tag}_junk", [128, 1024], BF16))
        ssq = [es.enter_context(nc.sbuf_tensor(f"{tag}_ssq{i}", [128, ns], F32)) for i in range(2)]
        pst = [es.enter_context(nc.psum_tensor(f"{tag}_pst{i}", [128, tw], BF16)) for i in range(2)]
        for tt in range(ntile):
            b = tt % 2
            kx, kn, kq = (tag, "xt", b), (tag, "xn", b), (tag, "ssq", b)
            self.dma(xt[b][:], xsrc[tt * tw:(tt + 1) * tw, :].rearrange("(s p) d -> p s d", p=128), [("x", tag)], [kx], "ld")
            S.add("dve", lambda e, b=b: e.memset(ssq[b][:], 0.0), writes=[kq])
            for s in range(ns):
                self.act(junk[:], xt[b][:, s, :], AF.Square, [kx, kq], [(tag, "junk"), kq], accum_out=ssq[b][:, s:s + 1])
            self.ts(ssq[b][:], ssq[b][:], 1.0 / 1024, 1e-6, ALU.mult, ALU.add, [kq], [kq])
            self.act(ssq[b][:], ssq[b][:], AF.Sqrt, [kq], [kq])
            S.add("dve", lambda e, b=b: e.reciprocal(out=ssq[b][:], in_=ssq[b][:]), reads=[kq], writes=[kq])
            for s in range(ns):
                if s % 2:
                    self.ts(xn[b][:, s, :], xt[b][:, s, :], ssq[b][:, s:s + 1], None, ALU.mult, None, [kx, kq], [kn])
                else:
                    S.add("act", lambda e, b=b, s=s: e.activation(out=xn[b][:, s, :], in_=xt[b][:, s, :], func=AF.Copy, scale=ssq[b][:, s:s + 1]), reads=[kx, kq], writes=[kn])
            for c in range(8):
                pb = c % 2
                kp = (tag, "pst", pb)
                for s in range(ns):
                    S.add("pe", lambda e, pb=pb, s=s, c=c, b=b: e.transpose(pst[pb][:, s * 128:(s + 1) * 128], xn[b][:, s, c * 128:(c + 1) * 128], self.identb[:]),
                          reads=[kn, "identb"], writes=[kp])
                eng = self.ev_eng()
                if eng == "act":
                    self.S.add("act", lambda e, pb=pb, c=c, tt=tt: e.activation(out=hT[:, c, tt * tw:(tt + 1) * tw], in_=pst[pb][:], func=AF.Copy, scale=gcol[:, c:c + 1]),
                               reads=[kp, "gT"], writes=[("hT", tt)])
                else:
                    self.ts(hT[:, c, tt * tw:(tt + 1) * tw], pst[pb][:], gcol[:, c:c + 1], None, ALU.mult, None, [kp, "gT"], [("hT", tt)])


    def post_residual(self, es, tag):
        nc, S = self.nc, self.S
        sb = lambda n, s, d: es.enter_context(nc.sbuf_tensor(f"{tag}_{n}", s, d))
        xr = [sb(f"xr{i}", [128, 1024], F32) for i in range(2)]
        tb = [sb(f"tb{i}", [128, 1024], F32) for i in range(2)]
        junk = sb("pjunk", [128, 512], BF16)
        sq = [sb(f"sq{i}", [128, 4], F32) for i in range(2)]
        st = {"n": 0}

        def fn(py0, py1, kpy, xsrc_rows, xdst_rows, gb, kx_r, kx_w):
            st["n"] += 1
            b = st["n"] % 2
            kxr, ktb, ksq = (tag, "xr", b), (tag, "tb", b), (tag, "sq", b)
            self.dma(xr[b][:], xsrc_rows, [kx_r], [kxr], "ld")
            S.add("dve", lambda e: e.memset(sq[b][:], 0.0), writes=[ksq])
            self.act(junk[:], py0, AF.Square, kpy + [ksq], [(tag, "junk"), ksq], accum_out=sq[b][:, 0:1])
            self.act(junk[:], py1, AF.Square, kpy + [ksq], [(tag, "junk"), ksq], accum_out=sq[b][:, 1:2])
            self.tt(sq[b][:, 2:3], sq[b][:, 0:1], sq[b][:, 1:2], ALU.add, [ksq], [ksq])
            self.ts(sq[b][:, 2:3], sq[b][:, 2:3], 1.0 / 1024, 1e-6, ALU.mult, ALU.add, [ksq], [ksq])
            self.act(sq[b][:, 2:3], sq[b][:, 2:3], AF.Sqrt, [ksq], [ksq])
            S.add("dve", lambda e: e.reciprocal(out=sq[b][:, 3:4], in_=sq[b][:, 2:3]), reads=[ksq], writes=[ksq])
            self.stt(tb[b][:, 0:512], py0, sq[b][:, 3:4], gb[:, 0:512], ALU.mult, ALU.mult, kpy + [ksq, "gb"], [ktb])
            self.stt(tb[b][:, 512:1024], py1, sq[b][:, 3:4], gb[:, 512:1024], ALU.mult, ALU.mult, kpy + [ksq, "gb"], [ktb])
            self.tt(tb[b][:], tb[b][:], xr[b][:], ALU.add, [ktb, kxr], [ktb], eng="pool")
            self.dma(xdst_rows, tb[b][:], [ktb], [kx_w], "sto")
        return fn

    def phase_ffn(self, l, xsrc, xdst, hT, gT, W):
        nc, S = self.nc, self.S
        with ExitStack() as es:
            self.rmsnorm_to_hT(es, xsrc, gT[:, l, 32:40], hT, f"n5_{l}")
            S.fence()
        actT = W["actT"]
        with ExitStack() as es:
            sb = lambda n, s, d: es.enter_context(nc.sbuf_tensor(f"{n}_G{l}", s, d))
            wg = [sb(f"wg{i}", [128, 8, 512], BF16) for i in range(2)]
            wu = [sb(f"wu{i}", [128, 8, 512], BF16) for i in range(2)]
            sg = [sb(f"sg{i}", [128, 512], F32) for i in range(2)]
            ab = [sb(f"ab{i}", [128, 512], BF16) for i in range(2)]
            pg = [es.enter_context(nc.psum_tensor(f"pg{i}_G{l}", [128, 512], F32)) for i in range(2)]
            pu = [es.enter_context(nc.psum_tensor(f"pu{i}_G{l}", [128, 512], F32)) for i in range(2)]
            wgv = W["ffn_w_gate"][l].rearrange("(k p) c -> p k c", p=128)
            wuv = W["ffn_w_up"][l].rearrange("(k p) c -> p k c", p=128)
            n = 0
            for grp in range(6):
                c0 = grp * 512
                ncol = min(512, 2816 - c0)
                b = grp % 2
                self.dma(wg[b][:, :, 0:ncol], wgv[:, :, c0:c0 + ncol], [], [("wg", b)], "ldw", q="pool")
                self.dma(wu[b][:, :, 0:ncol], wuv[:, :, c0:c0 + ncol], [], [("wu", b)], "ldw", q="pool")
                for fc in range(ncol // 128):
                    for tt in range(NT):
                        n += 1
                        p = n % 2
                        tsl = slice(tt * 512, (tt + 1) * 512)
                        for k in range(8):
                            self.mm(pg[p][:], wg[b][:, k, fc * 128:(fc + 1) * 128], hT[:, k, tsl], k == 0, k == 7, [("wg", b), ("hT", tt)], [("pg", p)])
                        for k in range(8):
                            self.mm(pu[p][:], wu[b][:, k, fc * 128:(fc + 1) * 128], hT[:, k, tsl], k == 0, k == 7, [("wu", b), ("hT", tt)], [("pu", p)])
                        self.act(sg[p][:], pg[p][:], AF.Silu, [("pg", p)], [("sg", p)])
                        self.tt(ab[p][:], sg[p][:], pu[p][:], ALU.mult, [("sg", p), ("pu", p)], [("ab", p)])
                        f0 = c0 + fc * 128
                        self.dma(actT[f0:f0 + 128, tsl], ab[p][:], [("ab", p)], [("actT", tt)], "st")
            S.fence()
        with ExitStack() as es:
            sb = lambda n, s, d: es.enter_context(nc.sbuf_tensor(f"{n}_H{l}", s, d))
            wd = sb("wd", [128, 22, 1024], BF16)
            at = [sb(f"at{i}", [128, 22, 512], BF16) for i in range(2)]
            gb = sb("gb", [128, 1024], F32)
            py = [es.enter_context(nc.psum_tensor(f"py{i}_H{l}", [128, 512], F32)) for i in range(4)]
            self.dma(wd[:], W["ffn_w_down"][l].rearrange("(k p) c -> p k c", p=128), [], ["wd"], "ldw", q="pool")
            self.dma(gb[:], W["norm_gains"][l, 5:6, :].partition_broadcast(128) if False else W["gbc"][l, 5], [], ["gb"], "ld")
            post = self.post_residual(es, f"prG{l}")
            n = 0
            for tt in range(NT):
                b = tt % 2
                self.dma(at[b][:], actT[:, tt * 512:(tt + 1) * 512].rearrange("(k p) t -> p k t", p=128), [("actT", tt)], [("at", b)], "ld")
                for s_ in range(4):
                    n += 1
                    pp = (n % 2) * 2
                    for half in range(2):
                        for k in range(22):
                            self.mm(py[pp + half][:], at[b][:, k, s_ * 128:(s_ + 1) * 128], wd[:, k, half * 512:(half + 1) * 512], k == 0, k == 21,
                                    [("at", b), "wd"], [("py", pp + half)])
                    t0 = tt * 512 + s_ * 128
                    post(py[pp][:], py[pp + 1][:], [("py", pp), ("py", pp + 1)], xsrc[t0:t0 + 128, :], xdst[t0:t0 + 128, :], gb, ("xd", tt), ("xo", tt))
            S.fence()

    def phase_xattn(self, l, xsrc, xdst, hT, gT, memg, W):
        nc, S = self.nc, self.S
        with ExitStack() as es:
            sb = lambda n, s, d: es.enter_context(nc.sbuf_tensor(f"{n}_F{l}", s, d))
            mT = sb("mT", [128, 8, 256], BF16)
            kxT = sb("kxT", [128, 2, 256], BF16)
            vaug = sb("vaug", [128, 2, 4, 128], BF16)
            wq = sb("wq", [128, 8, 256], BF16)
            wk = sb("wk", [128, 8, 256], BF16)
            wv = sb("wv", [128, 8, 256], BF16)
            wo = sb("wo", [64, 4, 1024], BF16)
            gb = sb("gb", [128, 1024], F32)
            qx = [sb(f"qx{i}", [128, 2, 512], BF16) for i in range(2)]
            pT = [sb(f"pT{i}", [128, 512], BF16) for i in range(3)]
            rc = [sb(f"rc{i}", [64, 512], F32) for i in range(2)]
            oT = [sb(f"oT{i}", [64, 4, 512], BF16) for i in range(2)]
            with ExitStack() as es2:
                self.rmsnorm_to_hT(es2, W["mem"], memg[:, l, :], mT, f"nm_{l}", ntok=256)
                S.fence()
            with ExitStack() as es2:
                self.rmsnorm_to_hT(es2, xsrc, gT[:, l, 16:24], hT, f"n3_{l}")
                S.fence()
            ps = [es.enter_context(nc.psum_tensor(f"ps{i}_F{l}", [128, 512], F32)) for i in range(3)]
            pa = [es.enter_context(nc.psum_tensor(f"pa{i}_F{l}", [128, 512], F32)) for i in range(2)]
            py = [es.enter_context(nc.psum_tensor(f"py{i}_F{l}", [128, 512], F32)) for i in range(2)]
            for nm, t_, src in (("wq", wq, "xa_w_q"), ("wk", wk, "xa_w_k"), ("wv", wv, "xa_w_v")):
                self.dma(t_[:], W[src][l].rearrange("(k p) c -> p k c", p=128), [], [nm], "ldw", q="pool")
            self.dma(wo[:], W["xa_w_o"][l].rearrange("(h p) c -> p h c", p=64), [], ["wo"], "ldw", q="pool")
            self.dma(gb[:], W["gbc"][l, 3], [], ["gb"], "ld")
            S.add("pool", lambda e: e.memset(vaug[:], 1.0), writes=["vaug"])
            for c in range(2):
                for k in range(8):
                    self.mm(ps[0][:, 0:256], wk[:, k, c * 128:(c + 1) * 128], mT[:, k, :], k == 0, k == 7, ["wk", ("hT", 0)], [("ps", 0)])
                self.copy(kxT[:, c, :], ps[0][:, 0:256], [("ps", 0)], ["kxT"])
            for mc in range(2):
                for k in range(8):
                    self.mm(ps[1][:, 0:256], mT[:, k, mc * 128:(mc + 1) * 128], wv[:, k, :], k == 0, k == 7, ["wv", ("hT", 0)], [("ps", 1)])
                self.copy(vaug[:, mc, :, 0:64], ps[1][:, 0:256].rearrange("p (h d) -> p h d", h=4), [("ps", 1)], ["vaug"])
            post = self.post_residual(es, f"prF{l}")
            n = 0
            for tt in range(NT):
                b = tt % 2
                tsl = slice(tt * 512, (tt + 1) * 512)
                for c in range(2):
                    for k in range(8):
                        self.mm(ps[2][:], wq[:, k, c * 128:(c + 1) * 128], hT[:, k, tsl], k == 0, k == 7, ["wq", ("hT", tt)], [("ps", 2)])
                    self.copy(qx[b][:, c, :], ps[2][:], [("ps", 2)], [("qx", b)])
                for h in range(4):
                    c, r0 = h // 2, (h % 2) * 64
                    a = h % 2
                    for mc in range(2):
                        n += 1
                        p = n % 2
                        self.mm(ps[p][:], kxT[r0:r0 + 64, c, mc * 128:(mc + 1) * 128], qx[b][r0:r0 + 64, c, :], True, True, ["kxT", ("qx", b)], [("ps", p)])
                        q_ = n % 3
                        self.act(pT[q_][:], ps[p][:], AF.Exp, [("ps", p)], [("pT", q_)], scale=0.125)
                        self.mm(pa[a][:], vaug[:, mc, h, :], pT[q_][:], mc == 0, mc == 1, ["vaug", ("pT", q_)], [("pa", a)])
                    S.add("dve", lambda e, a=a: e.reciprocal(out=rc[a][:], in_=pa[a][64:128, :]), reads=[("pa", a)], writes=[("rc", a)])
                    self.tt(oT[b][:, h, :], pa[a][0:64, :], rc[a][:], ALU.mult, [("pa", a), ("rc", a)], [("oT", b)])
                for s_ in range(4):
                    for half in range(2):
                        for h in range(4):
                            self.mm(py[half][:], oT[b][:, h, s_ * 128:(s_ + 1) * 128], wo[:, h, half * 512:(half + 1) * 512], h == 0, h == 3,
                                    [("oT", b), "wo"], [("py", half)])
                    t0 = tt * 512 + s_ * 128
                    post(py[0][:], py[1][:], [("py", 0), ("py", 1)], xsrc[t0:t0 + 128, :], xdst[t0:t0 + 128, :], gb, ("xd", tt), ("xo", tt))
            S.fence()


    def phase_merge(self, l, xsrc, xdst, hT, gT, W, w_in):
        nc, S = self.nc, self.S
        mergedT = W["mergedT"]
        with ExitStack() as es:
            sb = lambda n, s, d: es.enter_context(nc.sbuf_tensor(f"{n}_E{l}", s, d))
            yall = sb("yall", [128, 8, S_LEN], BF16)
            wmg = [sb(f"wmg{i}", [128, 3, 8, 128], BF16) for i in range(2)]
            wup = [sb(f"wup{i}", [128, 8, 128], BF16) for i in range(2)]
            sg = [sb(f"sg{i}", [128, 512], F32) for i in range(2)]
            acc = [sb(f"acc{i}", [128, 512], F32) for i in range(2)]
            mb = [sb(f"mb{i}", [128, 512], BF16) for i in range(2)]
            wo = sb("wo", [128, 8, 1024], BF16)
            gb = sb("gb", [128, 1024], F32)
            pg = [es.enter_context(nc.psum_tensor(f"pg{i}_E{l}", [128, 512], F32)) for i in range(2)]
            pu = [es.enter_context(nc.psum_tensor(f"pu{i}_E{l}", [128, 512], F32)) for i in range(2)]
            py = [es.enter_context(nc.psum_tensor(f"py{i}_E{l}", [128, 512], F32)) for i in range(2)]
            if "B" in self.phases:
                self.dma(yall[:, 0:4, :], W["ynsaT"].rearrange("(k p) t -> p k t", p=128), [], ["yall"], "ld")
            if "C" in self.phases:
                self.dma(yall[:, 4:6, :], W["yrwkvT"].rearrange("(k p) t -> p k t", p=128), [], ["yall"], "ld")
            if "D" in self.phases:
                self.dma(yall[:, 6:8, :], W["ys5T"].rearrange("(k p) t -> p k t", p=128), [], ["yall"], "ld")
            self.dma(wo[:], W["w_out"][l].rearrange("(k p) c -> p k c", p=128), [], ["wo"], "ldw", q="pool")
            self.dma(gb[:], W["gbc"][l, 1], [], ["gb"], "ld")
            wv = w_in[l].rearrange("(k p) c -> p k c", p=128)
            n = 0
            for f in range(8):
                b = f % 2
                for br in range(3):
                    c0 = C_MG + br * 1024 + f * 128
                    self.dma(wmg[b][:, br, :, :], wv[:, :, c0:c0 + 128], [], [("wmg", b)], "ldw", q="pool")
                fs = slice(f * 128, (f + 1) * 128)
                self.dma(wup[b][:, 0:4, :], W["w_up_nsa"][l].rearrange("(k p) c -> p k c", p=128)[:, :, fs], [], [("wup", b)], "ldw", q="pool")
                self.dma(wup[b][:, 4:6, :], W["w_up_rwkv"][l].rearrange("(k p) c -> p k c", p=128)[:, :, fs], [], [("wup", b)], "ldw", q="pool")
                self.dma(wup[b][:, 6:8, :], W["w_up_s5"][l].rearrange("(k p) c -> p k c", p=128)[:, :, fs], [], [("wup", b)], "ldw", q="pool")
                for tt in range(NT):
                    tsl = slice(tt * 512, (tt + 1) * 512)
                    a = tt % 2
                    brs = [(br, kk) for br, kk in enumerate(((0, 4), (4, 6), (6, 8))) if "BCD"[br] in self.phases]
                    for bi, (br, (k0, k1)) in enumerate(brs):
                        n += 1
                        p = n % 2
                        for k in range(8):
                            self.mm(pg[p][:], wmg[b][:, br, k, :], hT[:, k, tsl], k == 0, k == 7, [("wmg", b), ("hT", tt)], [("pg", p)])
                        for k in range(k0, k1):
                            self.mm(pu[p][:], wup[b][:, k, :], yall[:, k, tsl], k == k0, k == k1 - 1, [("wup", b), "yall"], [("pu", p)])
                        self.act(sg[p][:], pg[p][:], AF.Sigmoid, [("pg", p)], [("sg", p)])
                        last = bi == len(brs) - 1
                        if bi == 0:
                            self.tt(acc[a][:], sg[p][:], pu[p][:], ALU.mult, [("sg", p), ("pu", p)], [("acc", a)])
                            if last:
                                self.copy(mb[a][:], acc[a][:], [("acc", a)], [("mb", a)], eng="pool")
                        else:
                            self.tt(sg[p][:], sg[p][:], pu[p][:], ALU.mult, [("sg", p), ("pu", p)], [("sg", p)])
                            if not last:
                                self.tt(acc[a][:], acc[a][:], sg[p][:], ALU.add, [("acc", a), ("sg", p)], [("acc", a)], eng="pool")
                            else:
                                self.tt(mb[a][:], acc[a][:], sg[p][:], ALU.add, [("acc", a), ("sg", p)], [("mb", a)], eng="pool")
                    self.dma(mergedT[fs, tsl], mb[a][:], [("mb", a)], [("mergedT", tt)], "st")
            S.fence()
            self.dma(yall[:], mergedT.rearrange("(k p) t -> p k t", p=128), [], ["yall"], "ld")
            post = self.post_residual(es, f"prE{l}")
            for tt in range(NT):
                for s_ in range(4):
                    t0 = tt * 512 + s_ * 128
                    for half in range(2):
                        for k in range(8):
                            self.mm(py[half][:], yall[:, k, t0:t0 + 128], wo[:, k, half * 512:(half + 1) * 512], k == 0, k == 7, ["yall", "wo"], [("py", half)])
                    post(py[0][:], py[1][:], [("py", 0), ("py", 1)], xsrc[t0:t0 + 128, :], xdst[t0:t0 + 128, :], gb, ("xd", tt), ("xo", tt))
            S.fence()


    def phase_s5(self, l, W):
        nc, S = self.nc, self.S
        PI = math.pi
        uT, yT, ys5T = W["uT"], W["s5_yT"], W["ys5T"]
        with ExitStack() as es:
            sb = lambda n, s, d: es.enter_context(nc.sbuf_tensor(f"{n}_D{l}", s, d))
            prm = sb("prm", [128, 24, 8], F32)
            LR, LI, DT, RHOv, TH, CR, CI, ER, EI, T0, T1, T2, T3, SN, CS, IR, II = range(17)
            tau = sb("tau", [128, 512], F32)
            COS = sb("COS", [128, 8, 512], F32)
            SIN = sb("SIN", [128, 8, 512], F32)
            RHO = sb("RHO", [128, 8, 512], F32)
            Bb = sb("Bb", [32, 8, 2, 128], F32)
            Cb = sb("Cb", [128, 8, 2, 32], F32)
            dT = sb("dT", [32, 8], F32)
            ini = sb("ini", [128, 2, 8], F32)
            kp = "s5prm"
            P_ = lambda i: prm[:, i, :]
            self.dma(P_(LR), W["s5_lrT"][l], [], [kp], "ld")
            self.dma(P_(LI), W["s5_liT"][l], [], [kp], "ld")
            self.dma(P_(DT), W["s5_ldtT"][l], [], [kp], "ld")
            self.dma(tau[:], W["tau"], [], ["tau"], "ld")
            self.dma(Bb[:, :, 0, :], W["s5_BbT_re"][l].rearrange("j r c -> r j c"), [], ["Bb"], "ld")
            self.dma(Bb[:, :, 1, :], W["s5_BbT_im"][l].rearrange("j r c -> r j c"), [], ["Bb"], "ld")
            self.dma(Cb[:, :, 0, :], W["s5_CbT_re"][l].rearrange("j r c -> r j c"), [], ["Cb"], "ld")
            self.dma(Cb[:, :, 1, :], W["s5_CbT_im"][l].rearrange("j r c -> r j c"), [], ["Cb"], "ld")
            self.dma(dT[:], W["s5_dT"][l], [], ["dT"], "ld")
            S.add("dve", lambda e: e.memset(ini[:], 0.0), writes=["ini"])
            k1 = [kp]
            self.act(P_(DT), P_(DT), AF.Exp, k1, k1)
            self.tt(P_(T0), P_(LR), P_(DT), ALU.mult, k1, k1)
            self.act(P_(RHOv), P_(T0), AF.Exp, k1, k1)
            self.tt(P_(TH), P_(LI), P_(DT), ALU.mult, k1, k1)

            I32 = mybir.dt.int32
            it_s = sb("it_s", [128, 8], I32)
            it_b = sb("it_b", [128, 512], I32)
            tmpB = sb("tmpB", [128, 512], F32)

            def sincos(dst_s, dst_c, ang, keys_r, keys_w, tmp, tmp2, itmp):
                for dst, off in ((dst_s, 0.0), (dst_c, 0.25)):
                    self.ts(tmp, ang, 1.0 / (2 * PI), off, ALU.mult, ALU.add, keys_r, keys_w)
                    S.add("dve", lambda e, itmp=itmp, tmp=tmp: e.tensor_copy(out=itmp, in_=tmp), reads=keys_w, writes=keys_w)
                    S.add("dve", lambda e, itmp=itmp, tmp2=tmp2: e.tensor_copy(out=tmp2, in_=itmp), reads=keys_w, writes=keys_w)
                    self.tt(tmp, tmp, tmp2, ALU.subtract, keys_w, keys_w)
                    self.act(dst, tmp, AF.Sin, keys_w, keys_w, scale=2 * PI)

            sincos(P_(SN), P_(CS), P_(TH), k1, k1, P_(T0), P_(T3), it_s[:])
            self.tt(P_(T1), P_(RHOv), P_(CS), ALU.mult, k1, k1)
            self.tt(P_(T2), P_(RHOv), P_(SN), ALU.mult, k1, k1)
            self.ts(P_(T1), P_(T1), -1.0, None, ALU.add, None, k1, k1)
            self.tt(P_(T0), P_(LR), P_(LR), ALU.mult, k1, k1)
            self.tt(P_(T3), P_(LI), P_(LI), ALU.mult, k1, k1)
            self.tt(P_(T0), P_(T0), P_(T3), ALU.add, k1, k1)
            S.add("dve", lambda e: e.reciprocal(out=P_(T0), in_=P_(T0)), reads=k1, writes=k1)
            self.tt(P_(CR), P_(T1), P_(LR), ALU.mult, k1, k1)
            self.tt(P_(T3), P_(T2), P_(LI), ALU.mult, k1, k1)
            self.tt(P_(CR), P_(CR), P_(T3), ALU.add, k1, k1)
            self.tt(P_(CR), P_(CR), P_(T0), ALU.mult, k1, k1)
            self.tt(P_(CI), P_(T2), P_(LR), ALU.mult, k1, k1)
            self.tt(P_(T3), P_(T1), P_(LI), ALU.mult, k1, k1)
            self.tt(P_(CI), P_(CI), P_(T3), ALU.subtract, k1, k1)
            self.tt(P_(CI), P_(CI), P_(T0), ALU.mult, k1, k1)
            self.ts(P_(T1), P_(TH), 512.0, None, ALU.mult, None, k1, k1)
            sincos(P_(EI), P_(ER), P_(T1), k1, k1, P_(T0), P_(T3), it_s[:])
            tmpA = sb("tmpA", [128, 512], F32)
            for j in range(8):
                self.ts(RHO[:, j, :], tau[:], 0.0, prm[:, RHOv, j:j + 1], ALU.mult, ALU.add, ["tau", kp], ["RHO"])
                angj = sb(f"angj{j}", [128, 512], F32) if j == 0 else angj
                self.ts(angj[:], tau[:], prm[:, TH, j:j + 1], None, ALU.mult, None, ["tau", kp, "tmpA"], ["tmpA"])
                sincos(SIN[:, j, :], COS[:, j, :], angj[:], ["tmpA"], ["SIN", "COS", "tmpA"], tmpA[:], tmpB[:], it_b[:])
            NB = 2
            T = {nm: [sb(f"{nm}{i}", [128, 512], F32) for i in range(NB)] for nm in
                 ("t1", "t2", "inr", "ini_", "ma", "mb", "mr", "mi", "xr", "xi", "da", "db", "xre", "nxi")}
            uj = [sb(f"uj{i}", [32, 512], F32) for i in range(NB)]
            yj = [sb(f"yj{i}", [32, 512], F32) for i in range(NB)]
            sm = [sb(f"sm{i}", [128, 4], F32) for i in range(NB)]
            pb = [es.enter_context(nc.psum_tensor(f"pb{i}_D{l}", [128, 512], F32)) for i in range(4)]
            pyy = [es.enter_context(nc.psum_tensor(f"pyy{i}_D{l}", [32, 512], F32)) for i in range(2)]
            n = 0
            for tt in range(NT):
                tsl = slice(tt * 512, (tt + 1) * 512)
                for j in range(8):
                    n += 1
                    b = n % NB
                    K_ = lambda nm: (nm, b)
                    t = {nm: T[nm][b][:] for nm in T}
                    pr, pi_ = pb[(n % 2) * 2], pb[(n % 2) * 2 + 1]
                    kpr, kpi = ("pb", (n % 2) * 2), ("pb", (n % 2) * 2 + 1)
                    self.dma(uj[b][:], uT[32 * j:32 * j + 32, tsl], [], [K_("uj")], "ld")
                    self.mm(pr[:], Bb[:, j, 0, :], uj[b][:], True, True, ["Bb", K_("uj")], [kpr])
                    self.mm(pi_[:], Bb[:, j, 1, :], uj[b][:], True, True, ["Bb", K_("uj")], [kpi])
                    cr, ci = prm[:, CR, j:j + 1], prm[:, CI, j:j + 1]
                    self.ts(t["t1"], pi_[:], ci, None, ALU.mult, None, [kpi, kp], [K_("t1")])
                    self.stt(t["inr"], pr[:], cr, t["t1"], ALU.mult, ALU.subtract, [kpr, kp, K_("t1")], [K_("inr")])
                    self.ts(t["t2"], pr[:], ci, None, ALU.mult, None, [kpr, kp], [K_("t2")])
                    self.stt(t["ini_"], pi_[:], cr, t["t2"], ALU.mult, ALU.add, [kpi, kp, K_("t2")], [K_("ini_")])
                    self.tt(t["ma"], COS[:, j, :], t["inr"], ALU.mult, ["COS", K_("inr")], [K_("ma")], eng="pool")
                    self.tt(t["mb"], SIN[:, j, :], t["ini_"], ALU.mult, ["SIN", K_("ini_")], [K_("mb")], eng="pool")
                    self.tt(t["mr"], t["ma"], t["mb"], ALU.add, [K_("ma"), K_("mb")], [K_("mr")], eng="pool")
                    self.tt(t["ma"], COS[:, j, :], t["ini_"], ALU.mult, ["COS", K_("ini_")], [K_("ma")], eng="pool")
                    self.tt(t["mb"], SIN[:, j, :], t["inr"], ALU.mult, ["SIN", K_("inr")], [K_("mb")], eng="pool")
                    self.tt(t["mi"], t["ma"], t["mb"], ALU.subtract, [K_("ma"), K_("mb")], [K_("mi")], eng="pool")
                    S.add("dve", lambda e, t=t, j=j: e.tensor_tensor_scan(out=t["xr"], data0=RHO[:, j, :], data1=t["mr"], initial=ini[:, 0, j:j + 1], op0=ALU.mult, op1=ALU.add),
                          reads=["RHO", K_("mr"), "ini"], writes=[K_("xr")])
                    S.add("dve", lambda e, t=t, j=j: e.tensor_tensor_scan(out=t["xi"], data0=RHO[:, j, :], data1=t["mi"], initial=ini[:, 1, j:j + 1], op0=ALU.mult, op1=ALU.add),
                          reads=["RHO", K_("mi"), "ini"], writes=[K_("xi")])
                    er, ei = prm[:, ER, j:j + 1], prm[:, EI, j:j + 1]
                    xr_l, xi_l = T["xr"][b][:, 511:512], T["xi"][b][:, 511:512]
                    self.ts(sm[b][:, 0:1], xi_l, ei, None, ALU.mult, None, [K_("xi"), kp], [K_("sm")])
                    self.ts(sm[b][:, 1:2], xr_l, ei, None, ALU.mult, None, [K_("xr"), kp], [K_("sm")])
                    self.stt(ini[:, 0, j:j + 1], xr_l, er, sm[b][:, 0:1], ALU.mult, ALU.subtract, [K_("xr"), kp, K_("sm")], ["ini"])
                    self.stt(ini[:, 1, j:j + 1], xi_l, er, sm[b][:, 1:2], ALU.mult, ALU.add, [K_("xi"), kp, K_("sm")], ["ini"])
                    self.tt(t["da"], COS[:, j, :], t["xr"], ALU.mult, ["COS", K_("xr")], [K_("da")], eng="pool")
                    self.tt(t["db"], SIN[:, j, :], t["xi"], ALU.mult, ["SIN", K_("xi")], [K_("db")], eng="pool")
                    self.tt(t["xre"], t["da"], t["db"], ALU.subtract, [K_("da"), K_("db")], [K_("xre")], eng="pool")
                    self.tt(t["da"], SIN[:, j, :], t["xr"], ALU.mult, ["SIN", K_("xr")], [K_("da")], eng="pool")
                    self.tt(t["db"], COS[:, j, :], t["xi"], ALU.mult, ["COS", K_("xi")], [K_("db")], eng="pool")
                    self.stt(t["nxi"], t["da"], -1.0, t["db"], ALU.mult, ALU.subtract, [K_("da"), K_("db")], [K_("nxi")])
                    py_ = pyy[n % 2]
                    self.mm(py_[:], Cb[:, j, 0, :], t["xre"], True, False, ["Cb", K_("xre")], [("pyy", n % 2)])
                    self.mm(py_[:], Cb[:, j, 1, :], t["nxi"], False, True, ["Cb", K_("nxi")], [("pyy", n % 2)])
                    self.stt(yj[b][:], uj[b][:], dT[:, j:j + 1], py_[:], ALU.mult, ALU.add, [K_("uj"), "dT", ("pyy", n % 2)], [K_("yj")])
                    self.dma(yT[32 * j:32 * j + 32, tsl], yj[b][:], [K_("yj")], [("s5yT", tt)], "st")
            S.fence()
        with ExitStack() as es:
            sb = lambda n, s, d: es.enter_context(nc.sbuf_tensor(f"{n}_D2{l}", s, d))
            wgl = sb("wgl", [128, 2, 256], BF16)
            yv = [sb(f"yv{i}", [128, 2, 512], F32) for i in range(2)]
            tq = [sb(f"tq{i}", [128, 2, 512], F32) for i in range(2)]
            gf = [sb(f"gf{i}", [128, 2, 512], F32) for i in range(2)]
            gbf = [sb(f"gbf{i}", [128, 2, 512], BF16) for i in range(2)]
            ob = [sb(f"ob{i}", [128, 512], BF16) for i in range(2)]
            pgl = [es.enter_context(nc.psum_tensor(f"pgl{i}_D2{l}", [128, 512], F32)) for i in range(2)]
            self.dma(wgl[:], W["s5_w_glu"][l].rearrange("(k p) c -> p k c", p=128), [], ["wgl"], "ldw", q="pool")
            n = 0
            for tt in range(NT):
                b = tt % 2
                tsl = slice(tt * 512, (tt + 1) * 512)
                self.dma(yv[b][:], yT[:, tsl].rearrange("(k p) t -> p k t", p=128), [("s5yT", tt)], [("yv", b)], "ld")
                self.tt(tq[b][:], yv[b][:], yv[b][:], ALU.mult, [("yv", b)], [("tq", b)], eng="pool")
                self.ts(tq[b][:], tq[b][:], 0.044715, 1.0, ALU.mult, ALU.add, [("tq", b)], [("tq", b)])
                self.tt(tq[b][:], tq[b][:], yv[b][:], ALU.mult, [("tq", b), ("yv", b)], [("tq", b)], eng="pool")
                self.act(tq[b][:], tq[b][:], AF.Sigmoid, [("tq", b)], [("tq", b)], scale=1.5957691216057308)
                self.tt(gf[b][:], yv[b][:], tq[b][:], ALU.mult, [("tq", b), ("yv", b)], [("gf", b)])
                self.copy(gbf[b][:], gf[b][:], [("gf", b)], [("gbf", b)], eng="pool")
                for co in range(2):
                    n += 1
                    p = n % 2
                    for c in range(2):
                        self.mm(pgl[p][:], wgl[:, c, co * 128:(co + 1) * 128], gbf[b][:, c, :], c == 0, c == 1, ["wgl", ("gbf", b)], [("pgl", p)])
                    self.act(tq[b][:, co, :], pgl[p][:], AF.Sigmoid, [("pgl", p), ("tq", b), ("gf", b)], [("tq", b)])
                    self.tt(ob[p][:], gf[b][:, co, :], tq[b][:, co, :], ALU.mult, [("gf", b), ("tq", b)], [("ob", p)])
                    self.dma(ys5T[co * 128:(co + 1) * 128, tsl], ob[p][:], [("ob", p)], [("ys5T", tt)], "st")
            S.fence()


    def phase_nsa(self, l, W):
        nc, S = self.nc, self.S
        TINY = 1e-30
        with ExitStack() as es:
            sb = lambda n, s, d: es.enter_context(nc.sbuf_tensor(f"{n}_B{l}", s, d))
            kcT = sb("kcT", [64, 2, 256], BF16)
            vca = sb("vca", [128, 2, 2, 128], BF16)
            S.add("pool", lambda e: e.memset(vca[:], 1.0), writes=["vca"])
            with ExitStack() as es2:
                sb2 = lambda n, s, d: es2.enter_context(nc.sbuf_tensor(f"{n}_B0{l}", s, d))
                w1 = sb2("w1", [64, 32, 256], BF16)
                w2 = sb2("w2", [128, 2, 64], BF16)
                posT = sb2("posT", [64, 32], BF16)
                xin = sb2("xin", [64, 2, S_LEN], BF16)
                cpos = sb2("cpos", [128, 2], F32)
                hf = sb2("hf", [128, 256], F32)
                hq = sb2("hq", [128, 256], F32)
                hid = sb2("hid", [128, 2, 2, 256], BF16)
                ph = [es2.enter_context(nc.psum_tensor(f"ph{i}_B0{l}", [128, 256], F32)) for i in range(2)]
                pc = es2.enter_context(nc.psum_tensor(f"pc_B0{l}", [128, 2, 16], F32))
                for kv, (w1n, w2n, posn, src) in enumerate((("nsa_ck_w1", "nsa_ck_w2", "nsa_posT_k", W["kcmpT"]),
                                                             ("nsa_cv_w1", "nsa_cv_w2", "nsa_posT_v", W["vcmpT"]))):
                    self.dma(w1[:], W[w1n][l].rearrange("(l d) c -> d l c", d=64), [], ["w1"], "ldw", q="pool")
                    self.dma(w2[:], W[w2n][l].rearrange("(c p) d -> p c d", p=128), [], ["w2"], "ldw", q="pool")
                    self.dma(posT[:], W[posn][l], [], ["posT"], "ldw", q="pool")
                    self.dma(xin[:], src.rearrange("(g d) t -> d g t", d=64), [], ["xin"], "ld")
                    S.add("pool", lambda e: e.memset(hid[:], 0.0), writes=["hid"])
                    for c in range(2):
                        for li in range(32):
                            self.mm(pc[:, c, 0:1], w1[:, li, c * 128:(c + 1) * 128], posT[:, li:li + 1], li == 0, li == 31, ["w1", "posT"], ["pc"])
                    self.copy(cpos[:], pc[:, :, 0], ["pc"], ["cpos"], eng="dve")
                    n = 0
                    for g in range(2):
                        for c in range(2):
                            n += 1
                            p = n % 2
                            for li in range(32):
                                self.mm(ph[p][:, 0:255], w1[:, li, c * 128:(c + 1) * 128], xin[:, g, li:li + 16 * 254 + 1:16], li == 0, li == 31,
                                        ["w1", "xin"], [("ph", p)])
                            self.ts(hf[:, 0:255], ph[p][:, 0:255], cpos[:, c:c + 1], None, ALU.add, None, [("ph", p), "cpos"], ["hf"])
                            self.tt(hq[:, 0:255], hf[:, 0:255], hf[:, 0:255], ALU.mult, ["hf"], ["hq"])
                            self.ts(hq[:, 0:255], hq[:, 0:255], 0.044715, 1.0, ALU.mult, ALU.add, ["hq"], ["hq"])
                            self.tt(hq[:, 0:255], hq[:, 0:255], hf[:, 0:255], ALU.mult, ["hq", "hf"], ["hq"])
                            self.act(hq[:, 0:255], hq[:, 0:255], AF.Sigmoid, ["hq"], ["hq"], scale=1.5957691216057308)
                            self.tt(hid[:, c, g, 0:255], hf[:, 0:255], hq[:, 0:255], ALU.mult, ["hq", "hf"], ["hid"])
                    for g in range(2):
                        if kv == 0:
                            for c in range(2):
                                self.mm(ph[0][0:64, :], w2[:, c, :], hid[:, c, g, :], c == 0, c == 1, ["w2", "hid"], [("ph", 0)])
                            self.copy(kcT[:, g, :], ph[0][0:64, :], [("ph", 0)], ["kcT"], eng="dve")
                        else:
                            for nc_ in range(2):
                                for c in range(2):
                                    self.mm(ph[1][:, 0:64], hid[:, c, g, nc_ * 128:(nc_ + 1) * 128], w2[:, c, :], c == 0, c == 1, ["w2", "hid"], [("ph", 1)])
                                self.copy(vca[:, nc_, g, 0:64], ph[1][:, 0:64], [("ph", 1)], ["vca"], eng="dve")
                S.fence()
            import os
            self._nsa_dbg = int(os.environ.get("NSA_DBG", "9"))
            if self._nsa_dbg == 0:
                return
            Kaug = [sb(f"Kaug{g}", [128, S_LEN], BF16) for g in range(2)]
            Kwin = sb("Kwin", [64, 2, S_LEN], BF16)
            Vs = sb("Vs", [128, 32, 2, 128], BF16)
            Vw = sb("Vw", [128, 32, 2, 128], BF16)
            caus = sb("caus", [128, 4, 512], BF16)
            winm = sb("winm", [128, 8, 512], BF16)
            cmpm = sb("cmpm", [128, 2, 512], BF16)
            ovl = sb("ovl", [128, 2, 65], BF16)
            selhb = sb("selhb", [24, 24, 64], F32)
            gt = sb("gt", [24, 512], F32)
            Qa = [sb(f"Qa{r}", [128, 512], BF16) for r in range(4)]
            Qr = sb("Qr", [64, 4, 512], BF16)
            Pb = [sb(f"Pb{i}", [128, 512], BF16) for i in range(4)]
            oacc = sb("oacc", [64, 4, 512], F32)
            obf = sb("obf", [64, 4, 512], BF16)
            rc = sb("rc", [64, 512], F32)
            gm = sb("gm", [64, 512], F32)
            tmo = sb("tmo", [64, 512], F32)
            impa = sb("impa", [128, 4, 64], F32)
            m1 = sb("m1", [128, 4, 64], F32)
            am = sb("am", [128, 4, 64], F32)
            imf = sb("imf", [128, 64], F32)
            imw = sb("imw", [128, 64], F32)
            m8 = sb("m8", [128, 16], F32)
            dn = sb("dn", [128, 4], F32)
            nst = sb("nst", [128, 128], F32)
            ps = [es.enter_context(nc.psum_tensor(f"ps{i}_B{l}", [128, 512], F32)) for i in range(2)]
            pacc = [es.enter_context(nc.psum_tensor(f"pacc{i}_B{l}", [128, 512], F32)) for i in range(2)]
            pimp = es.enter_context(nc.psum_tensor(f"pimp_B{l}", [128, 4, 128], F32))
            pgb = es.enter_context(nc.psum_tensor(f"pgb_B{l}", [64, 512], F32))
            ptr = es.enter_context(nc.psum_tensor(f"ptr_B{l}", [128, 128], F32))
            for g in range(2):
                self.dma(Kaug[g][0:64, :], W["kselT"][g * 64:(g + 1) * 64, :], [], [("Kaug", g)], "ld")
                self.dma(Kaug[g][64:128, :], W["E64"], [], [("Kaug", g)], "ldc", q="pool")
            self.dma(Kwin[:], W["kwinT"].rearrange("(g d) t -> d g t", d=64), [], ["Kwin"], "ld")
            S.add("pool", lambda e: e.memset(Vs[:], 1.0), writes=["Vs"])
            S.add("pool", lambda e: e.memset(Vw[:], 1.0), writes=["Vw"])
            for g in range(2):
                self.dma(Vs[:, :, g, 0:64], W["vsel"][:, g * 64:(g + 1) * 64].rearrange("(k p) d -> p k d", p=128), ["Vs"], ["Vs"], "ld")
                self.dma(Vw[:, :, g, 0:64], W["vwin"][:, g * 64:(g + 1) * 64].rearrange("(k p) d -> p k d", p=128), ["Vw"], ["Vw"], "ld")
            self.dma(caus[:], W["CAUS"].rearrange("d p f -> p d f"), [], ["caus"], "ldc", q="pool")
            self.dma(winm[:], W["WINM"].rearrange("d p f -> p d f"), [], ["winm"], "ldc", q="pool")
            self.dma(ovl[:], W["OVL"].rearrange("(c p) j -> p c j", p=128), [], ["ovl"], "ldc", q="pool")
            self.dma(selhb[:], W["SELHB"], [], ["selhb"], "ld")
            S.add("dve", lambda e: e.memset(nst[:], 0.0), writes=["nst"])
            cnt = {"ps": 0, "pb": 0, "pa": 0}

            def finalize(pa, r, hb, first):
                kpa = ("pacc", pa)
                self.ts(rc[:], pacc[pa][64:128, :], TINY, None, ALU.max, None, [kpa], ["rc"])
                S.add("dve", lambda e: e.reciprocal(out=rc[:], in_=rc[:]), reads=["rc"], writes=["rc"])
                self.mm(pgb[:], selhb[:, hb, :], gt[:], True, True, ["selhb", "gt"], ["pgb"])
                self.tt(gm[:], pgb[:], rc[:], ALU.mult, ["pgb", "rc"], ["gm"])
                if first:
                    self.tt(oacc[:, r, :], pacc[pa][0:64, :], gm[:], ALU.mult, [kpa, "gm"], [("oacc", r)])
                else:
                    self.tt(tmo[:], pacc[pa][0:64, :], gm[:], ALU.mult, [kpa, "gm"], ["tmo"])
                    self.tt(oacc[:, r, :], oacc[:, r, :], tmo[:], ALU.add, [("oacc", r), "tmo"], [("oacc", r)], eng="pool")

            def score_tile(lhsT, rhs, mask, rd):
                cnt["ps"] += 1
                p = cnt["ps"] % 2
                self.mm(ps[p][:], lhsT, rhs, True, mask is None, rd, [("ps", p)])
                if mask is not None:
                    self.mm(ps[p][:], self.identb[:], mask[0], False, True, ["identb", mask[1]], [("ps", p)])
                cnt["pb"] += 1
                q_ = cnt["pb"] % 4
                self.act(Pb[q_][:], ps[p][:], AF.Exp, [("ps", p)], [("Pb", q_)], scale=0.125)
                return q_

            for qt in range(int(os.environ.get("NSA_NQT", "8")) if self._nsa_dbg >= 9 else 1):
                tsl = slice(qt * 512, (qt + 1) * 512)
                self.dma(cmpm[:], W["CMPM"][qt].rearrange("c p f -> p c f"), [], ["cmpm"], "ldc", q="pool")
                self.dma(gt[:], W["gatesT"][:, tsl], [], ["gt"], "ld")
                self.dma(m1[:], W["M1"][tsl, :].rearrange("(s p) j -> p s j", p=128), [], ["m1"], "ld")
                self.dma(am[:], W["ADDM"][tsl, :].rearrange("(s p) j -> p s j", p=128), [], ["am"], "ld")
                ncmp = 2 if qt >= 4 else 1
                for g in range(2):
                    for r in range(4):
                        h = 4 * g + r
                        self.dma(Qa[r][0:64, :], W["qT_rot"][h * 64:(h + 1) * 64, tsl], [], [("Qa", r)], "ld")
                        self.dma(Qr[:, r, :], W["qT_raw"][h * 64:(h + 1) * 64, tsl], [], [("Qr", r)], "ld")
                    for r in range(4):
                        h = 4 * g + r
                        cnt["pa"] += 1
                        pa = cnt["pa"] % 2
                        qs = []
                        for c in range(ncmp):
                            q_ = score_tile(kcT[:, g, c * 128:(c + 1) * 128], Qr[:, r, :], (cmpm[:, c, :], "cmpm"), ["kcT", ("Qr", r)])
                            qs.append(q_)
                            self.mm(pacc[pa][:], vca[:, c, g, :], Pb[q_][:], c == 0, c == ncmp - 1, ["vca", ("Pb", q_)], [("pacc", pa)])
                        for s_ in range(4):
                            for c in range(ncmp):
                                self.mm(pimp[:, s_, 0:65], Pb[qs[c]][:, s_ * 128:(s_ + 1) * 128], ovl[:, c, :], c == 0, c == ncmp - 1,
                                        [("Pb", qs[c]), "ovl"], ["pimp"])
                        self.ts(dn[:], pimp[:, :, 64], TINY, None, ALU.max, None, ["pimp"], ["dn"])
                        S.add("dve", lambda e: e.reciprocal(out=dn[:], in_=dn[:]), reads=["dn"], writes=["dn"])
                        for s_ in range(4):
                            if r == 0:
                                self.ts(impa[:, s_, :], pimp[:, s_, 0:64], dn[:, s_:s_ + 1], None, ALU.mult, None, ["pimp", "dn"], ["impa"])
                            else:
                                self.stt(impa[:, s_, :], pimp[:, s_, 0:64], dn[:, s_:s_ + 1], impa[:, s_, :], ALU.mult, ALU.add, ["pimp", "dn", "impa"], ["impa"])
                        finalize(pa, r, 3 * h + 0, True)
                    if self._nsa_dbg == 1:
                        continue
                    sub = int(os.environ.get("NSA_SUB", "9"))
                    for s_ in range(4):
                        self.tt(imf[:], impa[:, s_, :], m1[:, s_, :], ALU.mult, ["impa", "m1"], ["imf"])
                        self.tt(imf[:], imf[:], am[:, s_, :], ALU.add, ["imf", "am"], ["imf"])
                        if sub < 2:
                            continue
                        S.add("dve", lambda e: e.max(out=m8[:, 0:8], in_=imf[:]), reads=["imf"], writes=["m8"])
                        S.add("dve", lambda e: e.match_replace(out=imw[:], in_to_replace=m8[:, 0:8], in_values=imf[:], imm_value=-2e30), reads=["m8", "imf"], writes=["imw"])
                        S.add("dve", lambda e: e.max(out=m8[:, 8:16], in_=imw[:]), reads=["imw"], writes=["m8"])
                        if sub < 3:
                            continue
                        self.ts(imw[:], imf[:], m8[:, 15:16], BIG, ALU.is_ge, ALU.mult, ["imf", "m8"], ["imw"])
                        self.ts(nst[:, 64:128], imw[:], 1.0, -BIG, ALU.mult, ALU.add, ["imw"], ["nst"])
                        if sub < 4:
                            continue
                        S.add("pe", lambda e: e.transpose(ptr[:], nst[:], self.identf[:]), reads=["nst", "identf"], writes=["ptr"])
                        if sub < 5:
                            continue
                        for r in range(4):
                            self.copy(Qa[r][64:128, s_ * 128:(s_ + 1) * 128], ptr[64:128, :], ["ptr"], [("Qa", r)], eng=os.environ.get("CPE", "dve"))
                    if self._nsa_dbg == 2:
                        continue
                    for r in range(4):
                        h = 4 * g + r
                        cnt["pa"] += 1
                        pa = cnt["pa"] % 2
                        nk = 4 * qt + 4
                        for kt in range(nk):
                            mask = (caus[:, kt - 4 * qt, :], "caus") if kt >= 4 * qt else None
                            q_ = score_tile(Kaug[g][:, kt * 128:(kt + 1) * 128], Qa[r][:], mask, [("Kaug", g), ("Qa", r)])
                            self.mm(pacc[pa][:], Vs[:, kt, g, :], Pb[q_][:], kt == 0, kt == nk - 1, ["Vs", ("Pb", q_)], [("pacc", pa)])
                        finalize(pa, r, 3 * h + 1, False)
                        cnt["pa"] += 1
                        pa = cnt["pa"] % 2
                        k0 = max(0, 4 * qt - 4)
                        for kt in range(k0, nk):
                            q_ = score_tile(Kwin[:, g, kt * 128:(kt + 1) * 128], Qa[r][0:64, :], (winm[:, kt - 4 * qt + 4, :], "winm"), ["Kwin", ("Qa", r)])
                            self.mm(pacc[pa][:], Vw[:, kt, g, :], Pb[q_][:], kt == k0, kt == nk - 1, ["Vw", ("Pb", q_)], [("pacc", pa)])
                        finalize(pa, r, 3 * h + 2, False)
                        self.copy(obf[:, r, :], oacc[:, r, :], [("oacc", r)], [("obf", r)], eng="act")
                        self.dma(W["ynsaT"][h * 64:(h + 1) * 64, tsl], obf[:, r, :], [("obf", r)], [("ynsaT", qt)], "st")
            S.fence()


    def phase_rwkv(self, l, W):
        nc, S = self.nc, self.S
        G = 2
        NG = S_LEN // (64 * G)
        zr, yT = W["zrwkv"], W["yrwkvT"]
        with ExitStack() as es:
            sb = lambda n, s, d: es.enter_context(nc.sbuf_tensor(f"{n}_C{l}", s, d))
            bc = sb("bc", [64, 2816], F32)
            rwc = sb("rwc", [64, 1152], F32)
            Wwa = sb("Wwa", [128, 512], F32)
            g2 = sb("g2", [128, 256], F32)
            self.dma(bc[:], W["rw_bc"][l], [], ["bc"], "ld")
            self.dma(rwc[:], W["rwc"], [], ["rwc"], "ld")
            self.dma(Wwa[:], W["rw_Wwa"][l], [], ["Wwa"], "ld")
            self.dma(g2[:], W["rw_g2"][l], [], ["g2"], "ld")
            TRI, ONES = rwc[:, 0:64], rwc[:, 64:128]
            MASK1 = rwc[:, 128:640].rearrange("p (h n) -> p h n", h=4)
            MASK2 = rwc[:, 640:896].rearrange("p (h n) -> p h n", h=4)
            ID4 = rwc[:, 896:1152].rearrange("p (h n) -> p h n", h=4)
            idf = self.identf[0:64, 0:64]
            BC = lambda o, n: bc[:, o:o + n].unsqueeze(1).to_broadcast([64, G, n])
            mu_b, w0_b, a0_b, kk_b, ka_b, rk_b, lnw_b, lnb_b = (BC(0, 1024), BC(1024, 256), BC(1280, 256), BC(1536, 256), BC(1792, 256),
                                                              BC(2048, 256), BC(2304, 256), BC(2560, 256))
            z = sb("z", [64, G, 1024], F32)
            zp = sb("zp", [64, G, 1024], F32)
            t256 = lambda n: sb(n, [64, G, 256], F32)
            logw, al, gg, kkn, kp, bv, Linc, Ltot, T1, T2 = (t256(n) for n in ("logw", "al", "gg", "kkn", "kp", "bv", "Linc", "Ltot", "T1", "T2"))
            at, rt, bt, kt, Bh, Kh, ysb, bon = (t256(n) for n in ("at", "rt", "bt", "kt", "Bh", "Kh", "ysb", "bon"))
            st = sb("st", [64, G * 4, 4], F32)
            lT = sb("lT", [128, G, 2, 64], F32)
            CH = []
            for cp_ in range(2):
                CH.append(dict(
                    XT=sb(f"XT{cp_}", [64, 4, 4, 64], F32), Aab=sb(f"Aab{cp_}", [64, 4, 128], F32), Aak=sb(f"Aak{cp_}", [64, 4, 128], F32),
                    Pp=[sb(f"Pp{cp_}_{i}", [64, 4, 64], F32) for i in range(2)], Pt=[sb(f"Pt{cp_}_{i}", [64, 4, 64], F32) for i in range(2)],
                    Xi=sb(f"Xi{cp_}", [64, 4, 64], F32), AZ=sb(f"AZ{cp_}", [64, 4, 128], F32), AU=sb(f"AU{cp_}", [64, 4, 128], F32),
                    McT=sb(f"McT{cp_}", [64, 4, 64], F32), Ncs=sb(f"Ncs{cp_}", [64, 4, 64], F32), QhT=sb(f"QhT{cp_}", [64, 4, 64], F32),
                    WCT=sb(f"WCT{cp_}", [64, 4], F32)))
            ST = [sb(f"ST{i}", [64, 4, 64], F32) for i in range(2)]
            yob = sb("yob", [128, 2, 64 * G], BF16)
            PB = [es.enter_context(nc.psum_tensor(f"PB{i}_C{l}", [128, 512], F32)) for i in range(8)]
            cnt = {"pb": 0}

            def bank():
                cnt["pb"] += 1
                i = cnt["pb"] % 8
                return PB[i], ("PB", i)

            v4 = lambda ap: ap.rearrange("p (h n) -> p h n", h=4)
            S.add("dve", lambda e: e.memset(ST[0][:], 0.0), writes=[("ST", 0)])
            sidx = 0
            for gi in range(NG):
                t0 = gi * 64 * G
                self.dma(z[:], zr[t0:t0 + 64 * G, :].rearrange("(c p) d -> p c d", p=64), [], ["z"], "ld")
                if gi == 0:
                    S.add("dve", lambda e: e.memset(zp[0:1, 0, :], 0.0), writes=["zp"])
                    self.dma(zp[1:64, 0, :], zr[0:63, :], ["zp"], ["zp"], "ld")
                    self.dma(zp[:, 1:G, :], zr[63:63 + 64 * (G - 1), :].rearrange("(c p) d -> p c d", p=64), ["zp"], ["zp"], "ld")
                else:
                    self.dma(zp[:], zr[t0 - 1:t0 - 1 + 64 * G, :].rearrange("(c p) d -> p c d", p=64), [], ["zp"], "ld")
                self.tt(zp[:], zp[:], z[:], ALU.subtract, ["zp", "z"], ["zp"], eng="pool")
                self.tt(zp[:], zp[:], mu_b, ALU.mult, ["zp", "bc"], ["zp"])
                self.tt(z[:], z[:], zp[:], ALU.add, ["z", "zp"], ["z"], eng="pool")
                r_, k_, v_ = z[:, :, 0:256], z[:, :, 256:512], z[:, :, 512:768]
                pT, kpT = bank()
                pTv = pT[:, 0:G * 128].rearrange("p (c t n) -> p c t n", c=G, t=2)
                for c in range(G):
                    for t_ in range(2):
                        S.add("pe", lambda e, c=c, t_=t_, pTv=pTv: e.matmul(pTv[:, c, t_, :], z[:, c, 768 + 128 * t_:896 + 128 * t_], idf, start=True, stop=True), reads=["z", "identf"], writes=[kpT])
                self.act(lT[0:64, :, 0, :], pTv[0:64, :, 0, :], AF.Tanh, [kpT], ["lT"])
                self.act(lT[64:128, :, 0, :], pTv[64:128, :, 0, :], AF.Copy, [kpT], ["lT"])
                self.act(lT[:, :, 1, :], pTv[:, :, 1, :], AF.Sigmoid, [kpT], ["lT"])
                if gi == 0:
                    self.dump("lT", lT[:], [128, G, 2, 64], ["lT"])
                    self.dump("Wwa", Wwa[:], [128, 512], ["Wwa"])
                    self.dump("bc", bc[:], [64, 2816], ["bc"])
                for c in range(G):
                    pl, kpl = bank()
                    self.mm(pl[0:64, :], lT[:, c, 0, :], Wwa[:], True, True, ["lT", "Wwa"], [kpl])
                    self.tt(logw[:, c, :], pl[0:64, 0:256], bc[:, 1024:1280], ALU.add, [kpl, "bc"], ["logw"])
                    self.tt(al[:, c, :], pl[0:64, 256:512], bc[:, 1280:1536], ALU.add, [kpl, "bc"], ["al"])
                    pg_, kpg = bank()
                    self.mm(pg_[0:64, 0:256], lT[:, c, 1, :], g2[:], True, True, ["lT", "g2"], [kpg])
                    self.copy(gg[:, c, :], pg_[0:64, 0:256], [kpg], ["gg"], eng="dve")
                self.act(logw[:], logw[:], AF.Sigmoid, ["logw"], ["logw"])
                self.ts(logw[:], logw[:], -0.6065306597126334, None, ALU.mult, None, ["logw"], ["logw"])
                self.act(al[:], al[:], AF.Sigmoid, ["al"], ["al"])
                self.tt(kkn[:], k_, kk_b, ALU.mult, ["z", "bc"], ["kkn"], eng="pool")
                self.tt(T1[:], kkn[:], kkn[:], ALU.mult, ["kkn"], ["T1"])
                S.add("dve", lambda e: e.reduce_sum(out=st[:, :, 0].rearrange("p (c h) -> p c h", c=G), in_=T1[:].rearrange("p c (h n) -> p c h n", h=4), axis=AX.X), reads=["T1"], writes=["st"])
                self.ts(st[:, :, 0], st[:, :, 0], 1e-24, None, ALU.max, None, ["st"], ["st"])
                self.act(st[:, :, 0], st[:, :, 0], AF.Sqrt, ["st"], ["st"])
                S.add("dve", lambda e: e.reciprocal(out=st[:, :, 0], in_=st[:, :, 0]), reads=["st"], writes=["st"])
                st4 = lambda col: st[:, :, col].rearrange("p (c h) -> p c h", c=G)
                b16 = lambda col: st4(col).unsqueeze(3).to_broadcast([64, G, 4, 64])
                v16 = lambda ap: ap.rearrange("p c (h n) -> p c h n", h=4)
                self.tt(v16(kkn[:]), v16(kkn[:]), b16(0), ALU.mult, ["kkn", "st"], ["kkn"])
                self.ts(T1[:], al[:], -1.0, None, ALU.add, None, ["al"], ["T1"])
                self.tt(T1[:], T1[:], ka_b, ALU.mult, ["T1", "bc"], ["T1"])
                self.ts(T1[:], T1[:], 1.0, None, ALU.add, None, ["T1"], ["T1"])
                self.tt(kp[:], k_, T1[:], ALU.mult, ["z", "T1"], ["kp"], eng="pool")
                self.tt(bv[:], kkn[:], al[:], ALU.mult, ["kkn", "al"], ["bv"], eng="pool")
                self.tt(T2[:], r_, kp[:], ALU.mult, ["z", "kp"], ["T2"], eng="pool")
                self.tt(T2[:], T2[:], rk_b, ALU.mult, ["T2", "bc"], ["T2"])
                S.add("dve", lambda e: e.reduce_sum(out=st4(1), in_=v16(T2[:]), axis=AX.X), reads=["T2"], writes=["st"])
                self.tt(v16(bon[:]), v16(v_), b16(1), ALU.mult, ["z", "st"], ["bon"])
                for c in range(G):
                    pL, kpL = bank()
                    self.mm(pL[0:64, 0:256], TRI, logw[:, c, :], True, True, ["rwc", "logw"], [kpL])
                    self.mm(pL[0:64, 256:512], ONES, logw[:, c, :], True, True, ["rwc", "logw"], [kpL])
                    self.copy(Linc[:, c, :], pL[0:64, 0:256], [kpL], ["Linc"], eng="dve")
                    self.copy(Ltot[:, c, :], pL[0:64, 256:512], [kpL], ["Ltot"], eng="dve")
                self.tt(T1[:], Linc[:], logw[:], ALU.subtract, ["Linc", "logw"], ["T1"], eng="pool")
                self.act(T1[:], T1[:], AF.Exp, ["T1"], ["T1"])
                self.stt(at[:], kkn[:], -1.0, T1[:], ALU.mult, ALU.mult, ["kkn", "T1"], ["at"])
                self.act(T1[:], Linc[:], AF.Exp, ["Linc", "T1"], ["T1"])
                self.tt(rt[:], r_, T1[:], ALU.mult, ["z", "T1"], ["rt"], eng="pool")
                self.act(T1[:], Linc[:], AF.Exp, ["Linc", "T1"], ["T1"], scale=-1.0)
                self.tt(bt[:], bv[:], T1[:], ALU.mult, ["bv", "T1"], ["bt"])
                self.tt(kt[:], kp[:], T1[:], ALU.mult, ["kp", "T1"], ["kt"], eng="pool")
                self.tt(T1[:], Ltot[:], Linc[:], ALU.subtract, ["Ltot", "Linc", "T1"], ["T1"], eng="pool")
                self.act(T1[:], T1[:], AF.Exp, ["T1"], ["T1"])
                self.tt(Bh[:], bv[:], T1[:], ALU.mult, ["bv", "T1"], ["Bh"])
                self.tt(Kh[:], kp[:], T1[:], ALU.mult, ["kp", "T1"], ["Kh"], eng="pool")
                if gi == 0:
                    for nm, t_ in (("zs", z), ("logw", logw), ("al", al), ("gg", gg), ("kkn", kkn), ("kp", kp), ("bv", bv), ("Linc", Linc), ("Ltot", Ltot),
                                   ("at", at), ("rt", rt), ("bt", bt), ("kt", kt), ("Bh", Bh), ("Kh", Kh), ("bon", bon)):
                        self.dump(nm, t_[:], [64, G, t_.shape[2]], [nm if nm != "zs" else "z"])
                hs = lambda h: slice(h * 64, (h + 1) * 64)
                K2 = lambda n_, c: (n_, c)
                curs = [0] * G

                def tiles(c):
                    return tuple(CH[c][n_] for n_ in ("XT", "Aab", "Aak", "Pp", "Pt", "Xi", "AZ", "AU", "McT", "Ncs", "QhT", "WCT"))

                def st_xt(c):
                    XT, Aab, Aak, Pp, Pt, Xi, AZ, AU, McT, Ncs, QhT, WCT = tiles(c)
                    for half, (qa, qb) in enumerate((((at, "at"), (rt, "rt")), ((bt, "bt"), (kt, "kt")))):
                        pX, kpX = bank()
                        pXv = pX[0:64, :].rearrange("p (q h n) -> p q h n", q=2, h=4)
                        for qi, (src, skey) in enumerate((qa, qb)):
                            for h in range(4):
                                S.add("pe", lambda e, src=src, qi=qi, h=h, c=c, pXv=pXv: e.transpose(pXv[:, qi, h, :], src[:, c, h * 64:(h + 1) * 64], idf),
                                      reads=[skey, "identf"], writes=[kpX])
                        self.copy(XT[:, 2 * half:2 * half + 2, :, :], pXv, [kpX], [K2("XT", c)], eng="dve")
                    pW, kpW = bank()
                    for h in range(4):
                        self.mm(pW[0:64, h * 16:h * 16 + 1], logw[:, c, hs(h)], ONES[:, 0:1], True, True, ["logw", "rwc"], [kpW])
                    self.act(WCT[:], pW[0:64, 0:64].rearrange("p (h n) -> p h n", h=4)[:, :, 0], AF.Exp, [kpW], [K2("WCT", c)])

                def st_a(c):
                    XT, Aab, Aak, Pp, Pt, Xi, AZ, AU, McT, Ncs, QhT, WCT = tiles(c)
                    pA, kpA = bank()
                    pA2, kpA2 = bank()
                    pN, kpN = bank()
                    for h in range(4):
                        rhs_ar = XT[:, 0:2, h, :]
                        self.mm(v4(pA[0:64, :])[:, h, :], XT[:, 2, h, :], rhs_ar, True, True, [K2("XT", c)], [kpA])
                        self.mm(v4(pA2[0:64, :])[:, h, :], XT[:, 3, h, :], rhs_ar, True, True, [K2("XT", c)], [kpA2])
                        self.mm(v4(pN[0:64, 0:256])[:, h, :], XT[:, 0, h, :], XT[:, 2, h, :], True, True, [K2("XT", c)], [kpN])
                    self.tt(Aab[:], v4(pA[0:64, :]), MASK1, ALU.mult, [kpA, "rwc"], [K2("Aab", c)])
                    self.tt(Aak[:], v4(pA2[0:64, :]), MASK1, ALU.mult, [kpA2, "rwc"], [K2("Aak", c)])
                    self.tt(Pt[0][:], v4(pN[0:64, 0:256]), MASK2, ALU.mult, [kpN, "rwc"], [("Pt", c, 0)])
                    self.copy(Pp[0][:], Aab[:, :, 0:64], [K2("Aab", c)], [("Pp", c, 0)], eng="pool")
                    self.tt(Xi[:], Aab[:, :, 0:64], ID4, ALU.add, [K2("Aab", c), "rwc"], [K2("Xi", c)], eng="pool")
                    curs[c] = 0

                def st_lev(c):
                    XT, Aab, Aak, Pp, Pt, Xi, AZ, AU, McT, Ncs, QhT, WCT = tiles(c)
                    cur = curs[c]
                    nxt = 1 - cur
                    pP, kpP = bank()
                    pPv = pP[0:64, :].rearrange("p (q h n) -> p q h n", q=2, h=4)
                    for h in range(4):
                        self.mm(pPv[:, 0, h, :], Pt[cur][:, h, :], Pp[cur][:, h, :], True, True, [("Pt", c, cur), ("Pp", c, cur)], [kpP])
                        self.mm(pPv[:, 1, h, :], Pp[cur][:, h, :], Pt[cur][:, h, :], True, True, [("Pt", c, cur), ("Pp", c, cur)], [kpP])
                    self.copy(Pp[nxt][:], pPv[:, 0, :, :], [kpP], [("Pp", c, nxt)], eng="dve")
                    self.copy(Pt[nxt][:], pPv[:, 1, :, :], [kpP], [("Pt", c, nxt)], eng="dve")
                    pXi, kpXi = bank()
                    for h in range(4):
                        self.mm(v4(pXi[0:64, 0:256])[:, h, :], Pt[nxt][:, h, :], Xi[:, h, :], True, True, [("Pt", c, nxt), K2("Xi", c)], [kpXi])
                    self.tt(Xi[:], Xi[:], v4(pXi[0:64, 0:256]), ALU.add, [K2("Xi", c), kpXi], [K2("Xi", c)])
                    curs[c] = nxt

                def st_u(c):
                    XT, Aab, Aak, Pp, Pt, Xi, AZ, AU, McT, Ncs, QhT, WCT = tiles(c)
                    pZ, kpZ = bank()
                    for h in range(4):
                        self.mm(v4(pZ[0:64, 0:256])[:, h, :], Aak[:, h, 0:64], z[:, c, 512 + 64 * h:576 + 64 * h], True, True, [K2("Aak", c), "z"], [kpZ])
                    self.copy(AZ[:, :, 64:128], v4(pZ[0:64, 0:256]), [kpZ], [K2("AZ", c)], eng="dve")
                    self.copy(AZ[:, :, 0:64], v4(at[:, c, :]), ["at"], [K2("AZ", c)], eng="pool")
                    pU, kpU = bank()
                    for h in range(4):
                        self.mm(v4(pU[0:64, :])[:, h, :], Xi[:, h, :], AZ[:, h, :], True, True, [K2("Xi", c), K2("AZ", c)], [kpU])
                    self.copy(AU[:], v4(pU[0:64, :]), [kpU], [K2("AU", c)], eng="dve")

                def st_m(c):
                    XT, Aab, Aak, Pp, Pt, Xi, AZ, AU, McT, Ncs, QhT, WCT = tiles(c)
                    pM, kpM = bank()
                    pNc, kpNc = bank()
                    pQ, kpQ = bank()
                    for h in range(4):
                        vh = z[:, c, 512 + 64 * h:576 + 64 * h]
                        self.mm(v4(pM[0:64, 0:256])[:, h, :], AU[:, h, 0:64], Bh[:, c, hs(h)], True, True, [K2("AU", c), "Bh"], [kpM])
                        self.mm(v4(pNc[0:64, 0:256])[:, h, :], Bh[:, c, hs(h)], AU[:, h, 64:128], True, False, [K2("AU", c), "Bh"], [kpNc])
                        self.mm(v4(pNc[0:64, 0:256])[:, h, :], Kh[:, c, hs(h)], vh, False, True, ["Kh", "z"], [kpNc])
                        self.mm(v4(pQ[0:64, 0:256])[:, h, :], AU[:, h, 0:64], Aab[:, h, 64:128], True, True, [K2("AU", c), K2("Aab", c)], [kpQ])
                    for h in range(4):
                        self.stt(McT[:, h, :], idf, WCT[:, h:h + 1], v4(pM[0:64, 0:256])[:, h, :], ALU.mult, ALU.add, ["identf", K2("WCT", c), kpM], [K2("McT", c)])
                    self.copy(Ncs[:], v4(pNc[0:64, 0:256]), [kpNc], [K2("Ncs", c)], eng="dve")
                    self.tt(QhT[:], v4(pQ[0:64, 0:256]), XT[:, 1, :, :], ALU.add, [kpQ, K2("XT", c)], [K2("QhT", c)])

                for stage in [st_xt, st_a] + [st_lev] * 5 + [st_u, st_m]:
                    for c in range(G):
                        stage(c)
                for c in range(G):
                    XT, Aab, Aak, Pp, Pt, Xi, AZ, AU, McT, Ncs, QhT, WCT = tiles(c)
                    s0, s1 = ST[sidx], ST[1 - sidx]
                    k0, k1 = ("ST", sidx), ("ST", 1 - sidx)
                    pY, kpY = bank()
                    pS, kpS = bank()
                    for h in range(4):
                        vh = z[:, c, 512 + 64 * h:576 + 64 * h]
                        self.mm(v4(pY[0:64, 0:256])[:, h, :], Aab[:, h, 64:128], AU[:, h, 64:128], True, False, [K2("Aab", c), K2("AU", c)], [kpY])
                        self.mm(v4(pY[0:64, 0:256])[:, h, :], Aak[:, h, 64:128], vh, False, False, [K2("Aak", c), "z"], [kpY])
                        self.mm(v4(pY[0:64, 0:256])[:, h, :], QhT[:, h, :], s0[:, h, :], False, True, [K2("QhT", c), k0], [kpY])
                        self.mm(v4(pS[0:64, 0:256])[:, h, :], McT[:, h, :], s0[:, h, :], True, True, [K2("McT", c), k0], [kpS])
                    self.tt(s1[:], v4(pS[0:64, 0:256]), Ncs[:], ALU.add, [kpS, K2("Ncs", c)], [k1])
                    self.copy(ysb[:, c, :], pY[0:64, 0:256], [kpY], ["ysb"], eng="dve")
                    sidx = 1 - sidx
                S.add("dve", lambda e: e.reduce_sum(out=st4(2), in_=v16(ysb[:]), axis=AX.X), reads=["ysb"], writes=["st"])
                self.ts(st[:, :, 2], st[:, :, 2], -1.0 / 64, None, ALU.mult, None, ["st"], ["st"])
                self.tt(v16(ysb[:]), v16(ysb[:]), b16(2), ALU.add, ["ysb", "st"], ["ysb"])
                self.tt(T1[:], ysb[:], ysb[:], ALU.mult, ["ysb"], ["T1"], eng="pool")
                S.add("dve", lambda e: e.reduce_sum(out=st4(3), in_=v16(T1[:]), axis=AX.X), reads=["T1"], writes=["st"])
                self.ts(st[:, :, 3], st[:, :, 3], 1.0 / 64, 64e-5, ALU.mult, ALU.add, ["st"], ["st"])
                self.act(st[:, :, 3], st[:, :, 3], AF.Sqrt, ["st"], ["st"])
                S.add("dve", lambda e: e.reciprocal(out=st[:, :, 3], in_=st[:, :, 3]), reads=["st"], writes=["st"])
                self.tt(v16(ysb[:]), v16(ysb[:]), b16(3), ALU.mult, ["ysb", "st"], ["ysb"])
                self.tt(ysb[:], ysb[:], lnw_b, ALU.mult, ["ysb", "bc"], ["ysb"])
                self.tt(ysb[:], ysb[:], lnb_b, ALU.add, ["ysb", "bc"], ["ysb"])
                self.tt(ysb[:], ysb[:], bon[:], ALU.add, ["ysb", "bon"], ["ysb"], eng="pool")
                self.tt(ysb[:], ysb[:], gg[:], ALU.mult, ["ysb", "gg"], ["ysb"], eng="pool")
                pO, kpO = bank()
                pOv = pO[:, 0:2 * 64 * G].rearrange("p (k c n) -> p k c n", k=2, c=G)
                for c in range(G):
                    for kc in range(2):
                        S.add("pe", lambda e, c=c, kc=kc, pOv=pOv: e.matmul(pOv[:, kc, c, :], ysb[:, c, kc * 128:(kc + 1) * 128], idf, start=True, stop=True), reads=["ysb", "identf"], writes=[kpO])
                self.copy(yob[:], pO[:, 0:2 * 64 * G].rearrange("p (k t) -> p k t", k=2), [kpO], ["yob"], eng="act")
                for kc in range(2):
                    self.dma(yT[kc * 128:(kc + 1) * 128, t0:t0 + 64 * G], yob[:, kc, :], ["yob"], [("yrwkvT", gi)], "st")
            S.fence()

    def build(self):
        nc, S, L = self.nc, self.S, self.L
        din, dscr = self.din, self.dscr
        x_in = din("x", [S_LEN, D])
        mem = din("mem", [256, D])
        gT_d = din("gT", [L, 128, 48])
        memg_d = din("memgT", [L, 128, 8])
        w_in = din("w_in", [L, D, IN_W])
        identf_d = din("identf", [128, 128])
        pswap_d = din("pswap", [128, 128])
        cosT_d = din("cosT", [128, S_LEN])
        sinT_d = din("sinT", [128, S_LEN])
        out = nc.dram_tensor("out", [S_LEN, D], F32, kind="ExternalOutput").ap()
        W = {"mem": mem}
        for nm, shp in (("xa_w_q", [L, D, 256]), ("xa_w_k", [L, D, 256]), ("xa_w_v", [L, D, 256]), ("xa_w_o", [L, 256, D]),
                        ("ffn_w_gate", [L, D, 2816]), ("ffn_w_up", [L, D, 2816]), ("ffn_w_down", [L, 2816, D]),
                        ("w_up_nsa", [L, 512, D]), ("w_up_rwkv", [L, 256, D]), ("w_up_s5", [L, 256, D]), ("w_out", [L, D, D]),
                        ("gbc", [L, 6, 128, D])):
            W[nm] = din(nm, shp)
        W["actT"] = dscr("actT", [2816, S_LEN], BF16)
        W["s5_yT"] = dscr("s5_yT", [256, S_LEN], F32)
        for nm, shp in (("s5_lrT", [L, 128, 8]), ("s5_liT", [L, 128, 8]), ("s5_ldtT", [L, 128, 8]), ("tau", [128, 512]),
                        ("s5_BbT_re", [L, 8, 32, 128]), ("s5_BbT_im", [L, 8, 32, 128]), ("s5_CbT_re", [L, 8, 128, 32]),
                        ("s5_CbT_im", [L, 8, 128, 32]), ("s5_dT", [L, 32, 8]), ("s5_w_glu", [L, 256, 256])):
            W[nm] = din(nm, shp)
        W["mergedT"] = dscr("mergedT", [1024, S_LEN], BF16)
        W["ynsaT"] = dscr("ynsaT", [512, S_LEN], BF16)
        W["yrwkvT"] = dscr("yrwkvT", [256, S_LEN], BF16)
        W["ys5T"] = dscr("ys5T", [256, S_LEN], BF16)
        P = self.phases

        qT_raw = dscr("qT_raw", [512, S_LEN], BF16)
        qT_rot = dscr("qT_rot", [512, S_LEN], BF16)
        kcmpT = dscr("kcmpT", [128, S_LEN], BF16)
        vcmpT = dscr("vcmpT", [128, S_LEN], BF16)
        kselT = dscr("kselT", [128, S_LEN], BF16)
        kwinT = dscr("kwinT", [128, S_LEN], BF16)
        vsel = dscr("vsel", [S_LEN, 128], BF16)
        vwin = dscr("vwin", [S_LEN, 128], BF16)
        zrwkv = dscr("zrwkv", [S_LEN, 1024], F32)
        uT = dscr("uT", [256, S_LEN], F32)
        gatesT = dscr("gatesT", [24, S_LEN], F32)
        W["uT"] = uT
        W.update(kcmpT=kcmpT, vcmpT=vcmpT, kselT=kselT, kwinT=kwinT, vsel=vsel, vwin=vwin, qT_rot=qT_rot, qT_raw=qT_raw,
                 gatesT=gatesT, zrwkv=zrwkv)
        for nm, shp in (("rw_bc", [L, 64, 2816]), ("rwc", [64, 1152]), ("rw_Wwa", [L, 128, 512]), ("rw_g2", [L, 128, 256])):
            W[nm] = din(nm, shp)
        for nm, shp in (("nsa_ck_w1", [L, 2048, 256]), ("nsa_cv_w1", [L, 2048, 256]), ("nsa_ck_w2", [L, 256, 64]), ("nsa_cv_w2", [L, 256, 64]),
                        ("nsa_posT_k", [L, 64, 32]), ("nsa_posT_v", [L, 64, 32]), ("E64", [64, S_LEN]), ("CAUS", [4, 128, 512]),
                        ("WINM", [8, 128, 512]), ("CMPM", [8, 2, 128, 512]), ("OVL", [256, 65]), ("SELHB", [24, 24, 64]),
                        ("M1", [S_LEN, 64]), ("ADDM", [S_LEN, 64])):
            W[nm] = din(nm, shp)

        with ExitStack() as es0:
            sb0 = lambda n, s, d: es0.enter_context(nc.sbuf_tensor(n, s, d))
            self.identb = sb0("identb", [128, 128], BF16)
            self.identf = sb0("identf_sb", [128, 128], F32)
            self.pswap = sb0("pswap_sb", [128, 128], BF16)
            gT = sb0("gT_sb", [128, L, 48], F32)
            memg = sb0("memg_sb", [128, L, 8], F32)
            self.dma(memg[:], memg_d.rearrange("l p c -> p l c"), [], ["memg"], "ld")
            hT = sb0("hT", [128, 8, S_LEN], BF16)
            self.dma(self.identb[:], identf_d, [], ["identb"], "ldc", q="pool")
            self.dma(self.identf[:], identf_d, [], ["identf"], "ld")
            self.dma(self.pswap[:], pswap_d, [], ["pswap"], "ldc", q="pool")
            self.dma(gT[:], gT_d.rearrange("l p c -> p l c"), [], ["gT"], "ld")

            xcur = x_in
            for l in range(L):
                if "A" in P:
                  with ExitStack() as es:
                    self.rmsnorm_to_hT(es, xcur, gT[:, l, 0:8], hT, f"n1_{l}")
                    S.fence()
                with ExitStack() as es:
                  if "A" in P:
                    sb = lambda n, s, d: es.enter_context(nc.sbuf_tensor(f"{n}_A{l}", s, d))
                    wb = [sb(f"wb{i}", [128, 8, 512], BF16) for i in range(2)]
                    cosT = sb("cosT", [128, S_LEN], F32)
                    sinT = sb("sinT", [128, S_LEN], F32)
                    stg = [sb(f"stg{i}", [128, 512], F32) for i in range(3)]
                    stb = [sb(f"stb{i}", [128, 512], BF16) for i in range(4)]
                    pz = [es.enter_context(nc.psum_tensor(f"pz{i}_A{l}", [128, 512], F32)) for i in range(4)]
                    self.dma(cosT[:], cosT_d, [], ["cosT"], "ld")
                    self.dma(sinT[:], sinT_d, [], ["sinT"], "ld")
                    wv = w_in[l].rearrange("(k p) c -> p k c", p=128)
                    cnt = {"pz": 0, "stg": 0, "stb": 0, "wb": 0}

                    def nxt(name, n):
                        cnt[name] += 1
                        return cnt[name] % n

                    def load_w(c0, n):
                        b = nxt("wb", 2)
                        self.dma(wb[b][:, :, 0:n], wv[:, :, c0:c0 + n], [], [("wb", b)], "ldw", q="pool")
                        return b

                    def fm_chunk(b, off, nrow, tt):
                        p = nxt("pz", 4)
                        for k in range(8):
                            self.mm(pz[p][0:nrow, :], wb[b][:, k, off:off + nrow], hT[:, k, tt * 512:(tt + 1) * 512], k == 0, k == 7,
                                    [("wb", b), ("hT", tt)], [("pz", p)])
                        return p

                    def fm_plain(b, off, nrow, dst, bf=True):
                        for tt in range(NT):
                            p = fm_chunk(b, off, nrow, tt)
                            if bf:
                                s_ = nxt("stb", 4)
                                self.copy(stb[s_][0:nrow, :], pz[p][0:nrow, :], [("pz", p)], [("stb", s_)])
                                self.dma(dst[:, tt * 512:(tt + 1) * 512], stb[s_][0:nrow, :], [("stb", s_)], [("dst", id(dst))], "st")
                            else:
                                s_ = nxt("stg", 3)
                                self.copy(stg[s_][0:nrow, :], pz[p][0:nrow, :], [("pz", p)], [("stg", s_)])
                                self.dma(dst[:, tt * 512:(tt + 1) * 512], stg[s_][0:nrow, :], [("stg", s_)], [("dst", id(dst))], "st")

                    def fm_rope(b, off, dst_rot, dst_raw):
                        for tt in range(NT):
                            p = fm_chunk(b, off, 128, tt)
                            s_ = nxt("stb", 4)
                            self.copy(stb[s_][:], pz[p][:], [("pz", p)], [("stb", s_)])
                            if dst_raw is not None:
                                self.dma(dst_raw[:, tt * 512:(tt + 1) * 512], stb[s_][:], [("stb", s_)], [("dst", id(dst_raw))], "st")
                            p2 = nxt("pz", 4)
                            self.mm(pz[p2][:], self.pswap[:], stb[s_][:], True, True, ["pswap", ("stb", s_)], [("pz", p2)])
                            g1, g2 = nxt("stg", 3), nxt("stg", 3)
                            tsl = slice(tt * 512, (tt + 1) * 512)
                            self.tt(stg[g1][:], stb[s_][:], cosT[:, tsl], ALU.mult, [("stb", s_), "cosT"], [("stg", g1)], eng="pool")
                            self.tt(stg[g2][:], pz[p2][:], sinT[:, tsl], ALU.mult, [("pz", p2), "sinT"], [("stg", g2)])
                            s2 = nxt("stb", 4)
                            self.tt(stb[s2][:], stg[g1][:], stg[g2][:], ALU.add, [("stg", g1), ("stg", g2)], [("stb", s2)], eng="pool")
                            self.dma(dst_rot[:, tsl], stb[s2][:], [("stb", s2)], [("dst", id(dst_rot))], "st")

                    def tm_chunk(b, off, ncol, dst, dcol, bf):
                        for tt in range(NT):
                            for s in range(4):
                                t0 = tt * 512 + s * 128
                                p = nxt("pz", 4)
                                for k in range(8):
                                    self.mm(pz[p][:, 0:ncol], hT[:, k, t0:t0 + 128], wb[b][:, k, off:off + ncol], k == 0, k == 7,
                                            [("wb", b), ("hT", tt)], [("pz", p)])
                                if bf:
                                    s_ = nxt("stb", 4)
                                    self.copy(stb[s_][:, 0:ncol], pz[p][:, 0:ncol], [("pz", p)], [("stb", s_)])
                                    self.dma(dst[t0:t0 + 128, dcol:dcol + ncol], stb[s_][:, 0:ncol], [("stb", s_)], [("dst", id(dst))], "st")
                                else:
                                    s_ = nxt("stg", 3)
                                    self.copy(stg[s_][:, 0:ncol], pz[p][:, 0:ncol], [("pz", p)], [("stg", s_)])
                                    self.dma(dst[t0:t0 + 128, dcol:dcol + ncol], stg[s_][:, 0:ncol], [("stg", s_)], [("dst", id(dst))], "st")

                    b = load_w(C_Q, 512)
                    for c in range(4):
                        fm_rope(b, c * 128, qT_rot[c * 128:(c + 1) * 128, :], qT_raw[c * 128:(c + 1) * 128, :])
                    b = load_w(C_KCMP, 512)
                    fm_plain(b, 0, 128, kcmpT)
                    fm_plain(b, 128, 128, vcmpT)
                    fm_rope(b, 256, kselT, None)
                    tm_chunk(b, 384, 128, vsel, 0, True)
                    b = load_w(C_KWIN, 512)
                    fm_rope(b, 0, kwinT, None)
                    tm_chunk(b, 128, 128, vwin, 0, True)
                    tm_chunk(b, 256, 256, zrwkv, 0, False)
                    b = load_w(C_RWKV + 256, 512)
                    tm_chunk(b, 0, 512, zrwkv, 256, False)
                    b = load_w(C_RWKV + 768, 512)
                    tm_chunk(b, 0, 256, zrwkv, 768, False)
                    fm_plain(b, 256, 128, uT[0:128, :], bf=False)
                    fm_plain(b, 384, 128, uT[128:256, :], bf=False)
                    b = load_w(C_NG, 24)
                    for tt in range(NT):
                        p = fm_chunk(b, 0, 24, tt)
                        s_ = nxt("stg", 3)
                        self.act(stg[s_][0:24, :], pz[p][0:24, :], AF.Sigmoid, [("pz", p)], [("stg", s_)])
                        self.dma(gatesT[:, tt * 512:(tt + 1) * 512], stg[s_][0:24, :], [("stg", s_)], [("dst", id(gatesT))], "st")
                    S.fence()
                if "B" in P:
                    self.phase_nsa(l, W)
                if "C" in P:
                    self.phase_rwkv(l, W)
                if "D" in P and self.stop_after == "A":
                    self.phase_s5(l, W)
                if self.stop_after == "A":
                    break
                if "D" in P:
                    self.phase_s5(l, W)
                if "E" in P:
                    self.phase_merge(l, xcur, out, hT, gT, W, w_in)
                xmid = out if "E" in P else xcur
                if "F" in P:
                    self.phase_xattn(l, xmid, out, hT, gT, memg, W)
                if "G" in P:
                    self.phase_ffn(l, out, out, hT, gT, W)
                xcur = out
                S.fence()
                S.new_epoch()

            S.fence()
            S.emit(final_dma_groups=("st", "sto"))
        return nc


def host_consts():
    c = {}
    c["identf"] = np.eye(128, dtype=np.float32)
    ps = np.zeros((128, 128), np.float32)
    for m in range(128):
        hb, d = (m // 64) * 64, m % 64
        ps[hb + (d + 32) % 64, m] = 1.0
    c["pswap"] = ps
    inv = 1.0 / (10000.0 ** (np.arange(0, 64, 2, dtype=np.float32) / 64.0))
    ang = np.arange(S_LEN, dtype=np.float32)[None, :] * inv[:, None].astype(np.float32)
    cos, sin = np.cos(ang).astype(np.float32), np.sin(ang).astype(np.float32)
    cosT = np.zeros((128, S_LEN), np.float32)
    sinT = np.zeros((128, S_LEN), np.float32)
    for p in range(128):
        d = p % 64
        cosT[p] = cos[d % 32]
        sinT[p] = -sin[d % 32] if d < 32 else sin[d % 32]
    c["cosT"], c["sinT"] = cosT, sinT
    key = np.arange(S_LEN)
    c["E64"] = (key[None, :] // 64 == np.arange(64)[:, None]).astype(np.float32)
    p = np.arange(128)[:, None]
    f = np.arange(512)[None, :]
    c["CAUS"] = np.stack([np.where(128 * d + p <= f, 0.0, -BIG) for d in range(4)]).astype(np.float32)
    c["WINM"] = np.stack([np.where((128 * d + p <= f) & (128 * d + p > f - 512), 0.0, -BIG) for d in range(-4, 4)]).astype(np.float32)
    c["CMPM"] = np.stack([np.stack([np.where(16 * (128 * cc + p) + 31 <= 512 * qt + f, 0.0, -BIG) for cc in range(2)]) for qt in range(8)]).astype(np.float32)
    ncmp = 255
    cs = np.arange(ncmp) * 16
    ss = np.arange(64) * 64
    ov = ((cs[:, None] < ss[None, :] + 64) & (cs[:, None] + 32 > ss[None, :])).astype(np.float32)
    OVL = np.zeros((256, 65), np.float32)
    OVL[:255, :64] = ov
    OVL[:255, 64] = 1.0
    c["OVL"] = OVL
    sh = np.zeros((24, 24, 64), np.float32)
    for i in range(24):
        sh[i, i, :] = 1.0
    c["SELHB"] = sh
    t = np.arange(S_LEN)[:, None]
    blk = np.arange(64)[None, :]
    cur = t // 64
    forced = (blk == 0) | (blk == cur) | (blk == cur - 1)
    valid = blk <= cur
    c["M1"] = ((~forced) & valid).astype(np.float32)
    c["ADDM"] = (np.where(forced & valid, 1e9, 0.0) + np.where(valid, 0.0, -1e30)).astype(np.float32)
    jj = np.arange(64)[:, None]
    tt_ = np.arange(64)[None, :]
    tri = (jj <= tt_).astype(np.float32)
    m1 = np.concatenate([(jj < tt_).astype(np.float32), (jj <= tt_).astype(np.float32)], axis=1)
    m2 = (jj > tt_).astype(np.float32)
    c["rwc"] = np.ascontiguousarray(np.concatenate([tri, np.ones((64, 64), np.float32), np.tile(m1, (1, 4)), np.tile(m2, (1, 4)),
                                                    np.tile(np.eye(64, dtype=np.float32), (1, 4))], axis=1))
    c["tau"] = np.ascontiguousarray(np.broadcast_to(np.arange(512, dtype=np.float32)[None, :], (128, 512)))
    return c


def host_layout(inp, ls):
    m = {}
    w_in = inp["w_in"][ls]
    perm = np.concatenate([np.arange(0, 1280), np.arange(1304, 5656), np.arange(1280, 1304)])
    m["w_in"] = np.ascontiguousarray(w_in[:, :, perm])
    g = inp["norm_gains"][ls]
    m["gT"] = np.ascontiguousarray(g.reshape(len(ls), 6, 8, 128).transpose(0, 3, 1, 2).reshape(len(ls), 128, 48))
    m["memgT"] = np.ascontiguousarray(inp["mem_norm"][ls].reshape(len(ls), 8, 128).transpose(0, 2, 1))
    m["gbc"] = np.ascontiguousarray(np.broadcast_to(g[:, :, None, :], (len(ls), 6, 128, D)))
    nl = len(ls)
    for nm in ("nsa_ck_w1", "nsa_cv_w1", "nsa_ck_w2", "nsa_cv_w2"):
        m[nm] = np.ascontiguousarray(inp[nm][ls])
    m["nsa_posT_k"] = np.ascontiguousarray(inp["nsa_cmp_pos_k"][ls].transpose(0, 2, 1))
    m["nsa_posT_v"] = np.ascontiguousarray(inp["nsa_cmp_pos_v"][ls].transpose(0, 2, 1))
    vec = np.concatenate([inp["rwkv_mu"][ls], inp["rwkv_w0"][ls], inp["rwkv_a0"][ls], inp["rwkv_k_k"][ls], inp["rwkv_k_a"][ls],
                          inp["rwkv_r_k"][ls].reshape(nl, 256), inp["rwkv_ln_w"][ls], inp["rwkv_ln_b"][ls]], axis=1)
    m["rw_bc"] = np.ascontiguousarray(np.broadcast_to(vec[:, None, :], (nl, 64, 2816)))
    wwa = np.zeros((nl, 128, 512), np.float32)
    wwa[:, 0:64, 0:256] = inp["rwkv_w2"][ls]
    wwa[:, 64:128, 256:512] = inp["rwkv_a2"][ls]
    m["rw_Wwa"] = wwa
    m["rw_g2"] = np.ascontiguousarray(inp["rwkv_g2"][ls])
    def chan(a):
        return np.ascontiguousarray(a.reshape(nl, 8, 2, 64).transpose(0, 2, 3, 1).reshape(nl, 128, 8))
    m["s5_lrT"] = chan(inp["s5_lam_re"][ls])
    m["s5_liT"] = chan(inp["s5_lam_im"][ls])
    m["s5_ldtT"] = chan(np.broadcast_to(inp["s5_log_dt"][ls][:, :, None], (nl, 16, 64)))
    Bre, Bim = inp["s5_b_re"][ls], inp["s5_b_im"][ls]
    Cre, Cim = inp["s5_c_re"][ls], inp["s5_c_im"][ls]
    for nm, src in (("s5_BbT_re", Bre), ("s5_BbT_im", Bim)):
        o = np.zeros((nl, 8, 32, 128), np.float32)
        for j in range(8):
            for gl in range(2):
                o[:, j, gl * 16:(gl + 1) * 16, gl * 64:(gl + 1) * 64] = src[:, 2 * j + gl].transpose(0, 2, 1)
        m[nm] = o
    for nm, src in (("s5_CbT_re", Cre), ("s5_CbT_im", Cim)):
        o = np.zeros((nl, 8, 128, 32), np.float32)
        for j in range(8):
            for gl in range(2):
                o[:, j, gl * 64:(gl + 1) * 64, gl * 16:(gl + 1) * 16] = src[:, 2 * j + gl].transpose(0, 2, 1)
        m[nm] = o
    m["s5_dT"] = np.ascontiguousarray(inp["s5_d"][ls].reshape(nl, 8, 32).transpose(0, 2, 1))
    m["s5_w_glu"] = np.ascontiguousarray(inp["s5_w_glu"][ls])
    for nm in ("xa_w_q", "xa_w_k", "xa_w_v", "xa_w_o", "ffn_w_gate", "ffn_w_up", "ffn_w_down",
               "w_up_nsa", "w_up_rwkv", "w_up_s5", "w_out"):
        m[nm] = np.ascontiguousarray(inp[nm][ls])
    return m


_NC_CACHE = {}
PHASES = "ABCDEFG"


def get_nc(L, dbg=(), stop_after=None, phases="ABCDEFG"):
    key = (L, tuple(sorted(dbg)), stop_after, phases)
    if key not in _NC_CACHE:
        _NC_CACHE[key] = Builder(L, dbg, stop_after, phases).build()
    return _NC_CACHE[key]


N_LAYERS = 4
FUSED = True


def kernel(**inp):
    inp = {k: np.asarray(v) for k, v in inp.items()}
    B = inp["x"].shape[0]
    consts = host_consts()
    x = np.ascontiguousarray(inp["x"], dtype=np.float32)
    groups = [list(range(N_LAYERS))] if FUSED else [[l] for l in range(N_LAYERS)]
    for ls in groups:
        nc = get_nc(len(ls), phases=PHASES)
        lay = host_layout(inp, ls)
        in_maps = []
        for b in range(B):
            m = dict(consts)
            m.update(lay)
            m["x"] = x[b]
            m["mem"] = np.ascontiguousarray(inp["mem"][b])
            in_maps.append(m)
        res = run_bass_kernel_spmd(nc, in_maps, core_ids=list(range(B)))
        x = np.stack([r["out"] for r in res.results], axis=0)
    return x.astype(np.float32)
```
